# Optimizing a Trainium2 kernel written in Bass

```python
import math
import jax, jax.numpy as jnp
from jax import lax
import numpy as np

D_MODEL = 2048
BATCH = 2
SEQ = 16384
DEPTH = 1

MEM_LEN = 256
EPS = 1e-6
DA_HEADS = 8
DA_DK = 128
DA_DV = 256
Q_BLOCK = 128
ML_HEADS = 4
ML_D = 256
ML_CHUNK = 64
CONV_W = 4
XA_HEADS = 4
XA_D = 256
N_BRANCH = 3
N_GROUPS = 4
EXPERTS_PER_GROUP = 8
N_EXPERTS = N_GROUPS * EXPERTS_PER_GROUP
TOP_K = 2
D_EXPERT = 704
MOE_BLOCK = 128

SPLIT_SIZES = (
    DA_HEADS * 2 * DA_DK,
    DA_HEADS * 2 * DA_DK,
    DA_HEADS * DA_DV,
    2 * ML_HEADS * ML_D,
    ML_HEADS * ML_D,
    ML_HEADS * ML_D,
    2 * ML_HEADS,
    XA_HEADS * XA_D,
    N_BRANCH * D_MODEL,
)
D_IN = sum(SPLIT_SIZES)

kernel_name = "hybrid_diffattn_mlstm_memxattn_hmoe"


def rms_norm(x, g):
    x32 = x.astype(jnp.float32)
    y = x32 * lax.rsqrt(jnp.mean(x32 * x32, axis=-1, keepdims=True) + EPS)
    return (y * g.astype(jnp.float32)).astype(x.dtype)


def causal_conv(x, w, b):
    S = x.shape[1]
    xp = jnp.pad(x, ((0, 0), (CONV_W - 1, 0), (0, 0)))
    return sum(xp[:, j:j + S] * w[j] for j in range(CONV_W)) + b


def diff_attention(q, k, v, lam, slopes):
    B, S, H = q.shape[:3]
    q = jnp.transpose(q, (0, 2, 3, 1, 4)).astype(jnp.float32)
    k = jnp.transpose(k, (0, 2, 3, 1, 4)).astype(jnp.float32)
    v = jnp.transpose(v, (0, 2, 1, 3)).astype(jnp.float32)
    nb = S // Q_BLOCK
    qb = jnp.moveaxis(q.reshape(B, H, 2, nb, Q_BLOCK, DA_DK), 3, 0)
    key_pos = jnp.arange(S)
    scale = DA_DK ** -0.5

    def block(args):
        qi, i = args
        s = jnp.einsum('bhmqd,bhmkd->bhmqk', qi, k) * scale
        q_pos = i * Q_BLOCK + jnp.arange(Q_BLOCK)
        dist = q_pos[:, None] - key_pos[None, :]
        bias = -slopes[None, :, None, None, None] * dist.astype(jnp.float32)
        s = jnp.where(dist >= 0, s + bias, -jnp.inf)
        p = jax.nn.softmax(s, axis=-1)
        a = p[:, :, 0] - lam * p[:, :, 1]
        return jnp.einsum('bhqk,bhkd->bhqd', a, v)

    o = lax.map(block, (qb, jnp.arange(nb)))
    return jnp.transpose(o, (1, 0, 3, 2, 4)).reshape(B, S, H, DA_DV)


def mlstm_chunkwise(q, k, v, ig, lf):
    B, S, H, d = q.shape
    nc = S // ML_CHUNK

    def chunks(t):
        t = t.astype(jnp.float32).reshape((B, nc, ML_CHUNK, H) + t.shape[3:])
        return jnp.moveaxis(jnp.moveaxis(t, 1, 0), 3, 2)

    causal = jnp.tril(jnp.ones((ML_CHUNK, ML_CHUNK), dtype=bool))

    def step(carry, xs):
        C, n, m = carry
        qc, kc, vc, ic, fc = xs
        b = jnp.cumsum(fc, axis=-1)
        log_d = jnp.where(causal, b[..., :, None] - b[..., None, :] + ic[..., None, :], -jnp.inf)
        m_inter = b + m[..., None]
        m_t = jnp.maximum(m_inter, jnp.max(log_d, axis=-1))
        dmat = jnp.exp(log_d - m_t[..., None])
        inter = jnp.exp(m_inter - m_t)
        w = jnp.einsum('bhtd,bhsd->bhts', qc, kc) * dmat
        num = jnp.einsum('bhts,bhsd->bhtd', w, vc) + inter[..., None] * jnp.einsum('bhtd,bhde->bhte', qc, C)
        nq = jnp.sum(w, axis=-1) + inter * jnp.einsum('bhtd,bhd->bht', qc, n)
        h = num / jnp.maximum(jnp.abs(nq), jnp.exp(-m_t))[..., None]
        b_last = b[..., -1]
        a = b_last[..., None] - b + ic
        m_new = jnp.maximum(b_last + m, jnp.max(a, axis=-1))
        decay = jnp.exp(b_last + m - m_new)
        wa = jnp.exp(a - m_new[..., None])
        C_new = decay[..., None, None] * C + jnp.einsum('bhs,bhsd,bhse->bhde', wa, kc, vc)
        n_new = decay[..., None] * n + jnp.einsum('bhs,bhsd->bhd', wa, kc)
        return (C_new, n_new, m_new), h

    init = (jnp.zeros((B, H, d, d), jnp.float32), jnp.zeros((B, H, d), jnp.float32),
            jnp.zeros((B, H), jnp.float32))
    _, h = lax.scan(step, init, (chunks(q), chunks(k), chunks(v), chunks(ig), chunks(lf)))
    return jnp.transpose(h, (1, 0, 3, 2, 4)).reshape(B, S, H, d)


def memory_cross_attention(q, mk, mv):
    s = jnp.einsum('bshd,bmhd->bhsm', q.astype(jnp.float32), mk.astype(jnp.float32)) * (XA_D ** -0.5)
    p = jax.nn.softmax(s, axis=-1)
    return jnp.einsum('bhsm,bmhd->bshd', p, mv.astype(jnp.float32))


def hierarchical_moe(h, w_router_group, b_router_group, w_router_expert, b_router_expert,
                     w_expert_gate, w_expert_up, w_expert_down):
    B, S, D = h.shape
    T = B * S
    hf = h.reshape(T, D)
    g_logits = (hf @ w_router_group).astype(jnp.float32) + b_router_group.astype(jnp.float32)
    g_prob = jax.nn.softmax(g_logits, axis=-1)
    grp = jnp.argmax(g_logits, axis=-1)
    p_grp = jnp.take_along_axis(g_prob, grp[:, None], axis=-1)
    e_logits = ((hf @ w_router_expert).astype(jnp.float32)
                + b_router_expert.astype(jnp.float32)).reshape(T, N_GROUPS, EXPERTS_PER_GROUP)
    e_in = jnp.take_along_axis(e_logits, grp[:, None, None], axis=1)[:, 0]
    top_v, top_i = lax.top_k(e_in, TOP_K)
    wts = jax.nn.softmax(top_v, axis=-1) * p_grp
    eid = (grp[:, None] * EXPERTS_PER_GROUP + top_i).reshape(-1).astype(jnp.int32)
    tok = jnp.repeat(jnp.arange(T, dtype=jnp.int32), TOP_K)
    wflat = wts.reshape(-1)
    n_assign = T * TOP_K
    order = jnp.argsort(eid)
    eid_s, tok_s, w_s = eid[order], tok[order], wflat[order]
    counts = jax.ops.segment_sum(jnp.ones_like(eid), eid, num_segments=N_EXPERTS)
    start = jnp.cumsum(counts) - counts
    padded = (counts + MOE_BLOCK - 1) // MOE_BLOCK * MOE_BLOCK
    pad_end = jnp.cumsum(padded)
    pad_start = pad_end - padded
    dest = pad_start[eid_s] + (jnp.arange(n_assign, dtype=jnp.int32) - start[eid_s])
    nb = (n_assign + MOE_BLOCK - 1) // MOE_BLOCK + N_EXPERTS
    P = nb * MOE_BLOCK
    buf_tok = jnp.zeros((P,), jnp.int32).at[dest].set(tok_s)
    buf_w = jnp.zeros((P,), jnp.float32).at[dest].set(w_s)
    blk_e = jnp.minimum(jnp.searchsorted(pad_end, jnp.arange(nb, dtype=jnp.int32) * MOE_BLOCK, side='right'),
                        N_EXPERTS - 1)

    def expert_block(args):
        tk, wk, e = args
        xb = hf[tk]
        a = xb @ w_expert_gate[e]
        u = xb @ w_expert_up[e]
        return ((jax.nn.silu(a) * u) @ w_expert_down[e]) * wk[:, None].astype(xb.dtype)

    out = lax.map(expert_block, (buf_tok.reshape(nb, MOE_BLOCK), buf_w.reshape(nb, MOE_BLOCK), blk_e))
    y = jnp.zeros_like(hf).at[buf_tok].add(out.reshape(P, D))
    return y.reshape(B, S, D)


def hybrid_layer(x, mem, layer_idx, g_mix, w_in, conv_w, conv_b, b_igate, b_fgate,
                 lam_q1, lam_k1, lam_q2, lam_k2, g_diff_head, g_mlstm_head, g_mem, w_mem_kv,
                 w_branch_diff, w_branch_mlstm, w_branch_cross, b_gate, w_out, g_ffn,
                 w_router_group, b_router_group, w_router_expert, b_router_expert,
                 w_expert_gate, w_expert_up, w_expert_down):
    B, S, _ = x.shape
    h = rms_norm(x, g_mix)
    proj = h @ w_in
    points = np.cumsum(SPLIT_SIZES)[:-1].tolist()
    da_q, da_k, da_v, ml_qk, ml_v, ml_o, ml_g, xa_q, gate_pre = jnp.split(proj, points, axis=-1)

    lam_init = 0.8 - 0.6 * math.exp(-0.3 * layer_idx)
    lam = (jnp.exp(jnp.dot(lam_q1.astype(jnp.float32), lam_k1.astype(jnp.float32)))
           - jnp.exp(jnp.dot(lam_q2.astype(jnp.float32), lam_k2.astype(jnp.float32))) + lam_init)
    slopes = 2.0 ** (-8.0 * jnp.arange(1, DA_HEADS + 1, dtype=jnp.float32) / DA_HEADS)
    da = diff_attention(da_q.reshape(B, S, DA_HEADS, 2, DA_DK), da_k.reshape(B, S, DA_HEADS, 2, DA_DK),
                        da_v.reshape(B, S, DA_HEADS, DA_DV), lam, slopes)
    da = (rms_norm(da, g_diff_head) * (1.0 - lam_init)).reshape(B, S, DA_HEADS * DA_DV).astype(x.dtype)

    qk = jax.nn.silu(causal_conv(ml_qk, conv_w, conv_b))
    ml_q, ml_k = jnp.split(qk, 2, axis=-1)
    ig = ml_g[..., :ML_HEADS] + b_igate
    lf = jax.nn.log_sigmoid((ml_g[..., ML_HEADS:] + b_fgate).astype(jnp.float32))
    hm = mlstm_chunkwise(ml_q.reshape(B, S, ML_HEADS, ML_D), ml_k.reshape(B, S, ML_HEADS, ML_D) * (ML_D ** -0.5),
                         ml_v.reshape(B, S, ML_HEADS, ML_D), ig, lf)
    hm = (jax.nn.sigmoid(ml_o) * rms_norm(hm, g_mlstm_head).reshape(B, S, ML_HEADS * ML_D)).astype(x.dtype)

    mem_kv = rms_norm(mem, g_mem) @ w_mem_kv
    mk, mv = jnp.split(mem_kv, 2, axis=-1)
    M = mem.shape[1]
    xa = memory_cross_attention(xa_q.reshape(B, S, XA_HEADS, XA_D), mk.reshape(B, M, XA_HEADS, XA_D),
                                mv.reshape(B, M, XA_HEADS, XA_D))
    xa = xa.reshape(B, S, XA_HEADS * XA_D).astype(x.dtype)

    gates = jax.nn.sigmoid(gate_pre + b_gate).reshape(B, S, N_BRANCH, D_MODEL)
    merged = (gates[..., 0, :] * (da @ w_branch_diff) + gates[..., 1, :] * (hm @ w_branch_mlstm)
              + gates[..., 2, :] * (xa @ w_branch_cross))
    x = x + merged @ w_out

    x = x + hierarchical_moe(rms_norm(x, g_ffn), w_router_group, b_router_group, w_router_expert,
                             b_router_expert, w_expert_gate, w_expert_up, w_expert_down)
    return x


def setup_inputs(seed: int = 0) -> dict:
    key = jax.random.key(seed)
    ks = jax.random.split(key, 32)
    L, D = DEPTH, D_MODEL
    f32 = jnp.float32

    def nrm(k, shape, scale):
        return jax.random.normal(k, shape, f32) * scale

    def gain(k, shape):
        return 1.0 + 0.02 * jax.random.normal(k, shape, f32)

    f_bias_base = jnp.linspace(3.0, 6.0, ML_HEADS, dtype=f32)
    return {
        "x": nrm(ks[0], (BATCH, SEQ, D), 1.0),
        "mem": nrm(ks[1], (BATCH, MEM_LEN, D), 1.0),
        "g_mix": gain(ks[2], (L, D)),
        "w_in": nrm(ks[3], (L, D, D_IN), D ** -0.5),
        "conv_w": nrm(ks[4], (L, CONV_W, 2 * ML_HEADS * ML_D), CONV_W ** -0.5),
        "conv_b": nrm(ks[5], (L, 2 * ML_HEADS * ML_D), 0.02),
        "b_igate": nrm(ks[6], (L, ML_HEADS), 0.1),
        "b_fgate": f_bias_base + nrm(ks[7], (L, ML_HEADS), 0.1),
        "lam_q1": nrm(ks[8], (L, DA_DK), 0.1),
        "lam_k1": nrm(ks[9], (L, DA_DK), 0.1),
        "lam_q2": nrm(ks[10], (L, DA_DK), 0.1),
        "lam_k2": nrm(ks[11], (L, DA_DK), 0.1),
        "g_diff_head": gain(ks[12], (L, DA_HEADS, DA_DV)),
        "g_mlstm_head": gain(ks[13], (L, ML_HEADS, ML_D)),
        "g_mem": gain(ks[14], (L, D)),
        "w_mem_kv": nrm(ks[15], (L, D, 2 * XA_HEADS * XA_D), D ** -0.5),
        "w_branch_diff": nrm(ks[16], (L, DA_HEADS * DA_DV, D), (DA_HEADS * DA_DV) ** -0.5),
        "w_branch_mlstm": nrm(ks[17], (L, ML_HEADS * ML_D, D), (ML_HEADS * ML_D) ** -0.5),
        "w_branch_cross": nrm(ks[18], (L, XA_HEADS * XA_D, D), (XA_HEADS * XA_D) ** -0.5),
        "b_gate": nrm(ks[19], (L, N_BRANCH * D), 0.02),
        "w_out": nrm(ks[20], (L, D, D), D ** -0.5),
        "g_ffn": gain(ks[21], (L, D)),
        "w_router_group": nrm(ks[22], (L, D, N_GROUPS), D ** -0.5),
        "b_router_group": nrm(ks[23], (L, N_GROUPS), 0.01),
        "w_router_expert": nrm(ks[24], (L, D, N_EXPERTS), D ** -0.5),
        "b_router_expert": nrm(ks[25], (L, N_EXPERTS), 0.01),
        "w_expert_gate": nrm(ks[26], (L, N_EXPERTS, D, D_EXPERT), D ** -0.5),
        "w_expert_up": nrm(ks[27], (L, N_EXPERTS, D, D_EXPERT), D ** -0.5),
        "w_expert_down": nrm(ks[28], (L, N_EXPERTS, D_EXPERT, D), D_EXPERT ** -0.5),
        "g_final": gain(ks[29], (D,)),
    }


def reference(x, mem, g_mix, w_in, conv_w, conv_b, b_igate, b_fgate, lam_q1, lam_k1, lam_q2, lam_k2,
              g_diff_head, g_mlstm_head, g_mem, w_mem_kv, w_branch_diff, w_branch_mlstm, w_branch_cross,
              b_gate, w_out, g_ffn, w_router_group, b_router_group, w_router_expert, b_router_expert,
              w_expert_gate, w_expert_up, w_expert_down, g_final):
    for l in range(DEPTH):
        x = hybrid_layer(x, mem, l, g_mix[l], w_in[l], conv_w[l], conv_b[l], b_igate[l], b_fgate[l],
                         lam_q1[l], lam_k1[l], lam_q2[l], lam_k2[l], g_diff_head[l], g_mlstm_head[l],
                         g_mem[l], w_mem_kv[l], w_branch_diff[l], w_branch_mlstm[l], w_branch_cross[l],
                         b_gate[l], w_out[l], g_ffn[l], w_router_group[l], b_router_group[l],
                         w_router_expert[l], b_router_expert[l], w_expert_gate[l], w_expert_up[l],
                         w_expert_down[l])
    return rms_norm(x, g_final)
```

```python
import numpy as np
import concourse.bass as bass
import concourse.mybir as mybir
from concourse.bass_utils import run_bass_kernel_spmd

F32 = mybir.dt.float32
BF16 = mybir.dt.bfloat16
I32 = mybir.dt.int32
AF = mybir.ActivationFunctionType
ALU = mybir.AluOpType
AX = mybir.AxisListType

D = 2048
NCORES = 8
EPS = 1e-6
DEXP = 704
NEXP = 32
LAM_INIT = 0.8 - 0.6 * 1.0
WIN_CUT = 100.0
SLOPES = [2.0 ** (-8.0 * (h + 1) / 8) for h in range(8)]
HEAD_PAIRS = [(0, 7), (1, 6), (2, 5), (3, 4)]


class Buf:
    def __init__(self, ap, name):
        self.ap = ap
        self.name = name
        self.last_w = []
        self.readers = []
        self.dsem = None
        self.dcount = 0
        self.psum = False

    def __getitem__(self, k):
        return self.ap[k]


class Ctx:
    SEM_LIMIT = 30000

    dbg_names = ()

    def __init__(self, nc):
        self.nc = nc
        self.eng = {"pe": nc.tensor, "act": nc.scalar, "dve": nc.vector, "pool": nc.gpsimd, "sp": nc.sync}
        self.sem = {}
        self.cnt = {}
        self.nsem = 0
        for e in self.eng:
            self._new_sem(e)
        self.waited = {e: {} for e in self.eng}
        self.stack = []
        self.allbufs = []
        self.sem_pool = []
        self.uid = 0
        self.tot = {}

    def _new_sem(self, e):
        s = self.nc.semaphore(f"s_{e}_{self.nsem}")
        self.nsem += 1
        self.sem[e] = s.__enter__()
        self.cnt[e] = 0

    def sb(self, name, shape, dt):
        self.uid += 1
        cm = self.nc.sbuf_tensor(f"sb_{name}_{self.uid}", shape, dt)
        t = cm.__enter__()
        b = Buf(t, name)
        self.stack.append((cm, b))
        self.allbufs.append(b)
        return b

    def mark(self):
        return len(self.stack)

    def barrier(self):
        evs = []
        for e in ("pe", "act", "dve", "pool"):
            if self.cnt[e] > 0:
                evs.append((self.sem[e], self.cnt[e], None))
        for b in self.allbufs:
            if b.dsem is not None and b.dcount > 0:
                evs.append((b.dsem, b.dcount, None))
        for e in self.eng:
            self._wait(e, evs)

    def release(self, mark):
        self.barrier()
        while len(self.stack) > mark:
            cm, b = self.stack.pop()
            cm.__exit__(None, None, None)
            if b.dsem is not None:
                if b.dcount < 20000:
                    self.sem_pool.append((b.dsem, b.dcount))
                b.dsem = None
            if b in self.allbufs:
                self.allbufs.remove(b)

    def ps(self, name, shape, dt):
        t = self.nc.psum_tensor("pp_" + name, shape, dt).__enter__()
        b = Buf(t, name)
        b.psum = True
        return b

    def dram(self, name, shape, dt, kind="Internal"):
        if kind == "Internal" and name in self.dbg_names:
            kind = "ExternalOutput"
        t = self.nc.dram_tensor(name, shape, dt, kind=kind)
        b = Buf(t.ap(), name)
        self.allbufs.append(b)
        return b

    def _wait(self, e, events):
        eng = self.eng[e]
        w = self.waited[e]
        for (sem, val, src) in events:
            if src == "pe" and e == "pe":
                continue
            key = id(sem)
            if w.get(key, (None, 0))[1] >= val:
                continue
            eng.wait_ge(sem, val)
            w[key] = (sem, val)

    def _deps(self, reads, writes):
        ev = []
        for b in reads:
            ev += b.last_w
        for b in writes:
            ev += b.last_w + b.readers
        return ev

    def op(self, e, fn, reads=(), writes=()):
        pr = [b for b in reads if b.psum and b not in writes]
        if pr:
            writes = list(writes) + pr
            reads = [b for b in reads if not b.psum]
        self._wait(e, self._deps(reads, writes))
        ins = fn()
        if self.cnt[e] >= self.SEM_LIMIT:
            self._new_sem(e)
        self.cnt[e] += 1
        self.tot[e] = self.tot.get(e, 0) + 1
        ins.then_inc(self.sem[e], 1)
        evt = (self.sem[e], self.cnt[e], e)
        for b in writes:
            b.last_w = [evt]
            b.readers = []
        for b in reads:
            if b in writes:
                continue
            b.readers = [x for x in b.readers if x[2] != e or x[0] is not evt[0]] + [evt]
        return ins

    def dma(self, q, out, in_, reads=(), writes=(), multi=False, owner=None, **kw):
        ev = []
        for b in reads:
            ev += b.last_w
        for b in writes:
            if multi:
                ev += b.readers
            else:
                ev += b.last_w + b.readers
        self._wait(q, ev)
        tgt = owner if owner is not None else writes[0]
        if tgt.dsem is None:
            if self.sem_pool:
                tgt.dsem, tgt.dcount = self.sem_pool.pop()
            else:
                s = self.nc.semaphore(f"d_{self.nsem}")
                self.nsem += 1
                tgt.dsem = s.__enter__()
        ins = self.eng[q].dma_start(out=out, in_=in_, **kw)
        tgt.dcount += 16
        ins.then_inc(tgt.dsem, 16)
        evt = (tgt.dsem, tgt.dcount, None)
        for b in writes:
            if multi:
                b.last_w = [x for x in b.last_w if x[0] is not tgt.dsem] + [evt]
            else:
                b.last_w = [evt]
                b.readers = []
        for b in reads:
            b.readers = [x for x in b.readers if x[0] is not tgt.dsem] + [evt]
        return ins

    def drain(self, e, bufs):
        ev = []
        for b in bufs:
            ev += b.last_w + b.readers
        self._wait(e, ev)


def head_cols(g):
    hs = HEAD_PAIRS[g]
    cols = []
    for h in hs:
        cols += list(range(h * 256, h * 256 + 256))
    for h in hs:
        cols += list(range(2048 + h * 256, 2048 + h * 256 + 256))
    for h in hs:
        cols += list(range(4096 + h * 256, 4096 + h * 256 + 256))
    cols += list(range(6144 + g * 256, 6144 + g * 256 + 256))
    cols += list(range(6144 + 1024 + g * 256, 6144 + 1024 + g * 256 + 256))
    cols += list(range(8192 + g * 256, 8192 + g * 256 + 256))
    cols += list(range(9216 + g * 256, 9216 + g * 256 + 256))
    cols += [10240 + g, 10240 + 4 + g]
    return np.array(cols)


NA = 2562


def kb_list(h, t, NBLK):
    slope = SLOPES[h]
    out = []
    for kb in range(0, 2 * t + 2):
        dmin = (2 * t) * 128 - (kb * 128 + 127)
        if dmin > 0 and slope * dmin > WIN_CUT:
            continue
        out.append(kb)
    return out


def build(S, dbg=False, full=True):
    nc = bass.Bass("TRN2", target_bir_lowering=False)
    C = Ctx(nc)
    if dbg:
        C.dbg_names = dbg
    op, dma = C.op, C.dma
    NT = S // 512
    NBLK = S // 128
    SEG = S // 4
    NTB = SEG // 512

    def din(name, shape, dt=F32):
        return C.dram(name, shape, dt, kind="ExternalInput")

    x_d = din("x", [S, D])
    xseg_d = din("xseg", [SEG, D])
    mem_d = din("mem", [256, D])
    wA_d = din("wA", [D, NA])
    wB_d = din("wB", [D, 7168])
    wmem_d = din("wmem", [D, 2048])
    wbr_d = din("wbr", [4096, D])
    wout_d = din("wout", [D, D])
    wr_d = din("wr", [D, 36])
    if full:
        weg_d = din("weg", [NEXP, D, DEXP])
        weu_d = din("weu", [NEXP, D, DEXP])
        wed_d = din("wed", [NEXP, DEXP, D])
    gb_d = din("gb", [128, 4, D])
    hb_d = din("hb", [128, 768])
    fm_d = din("fm", [128, 64])
    lam_d = din("lam", [128, 512])
    brt_d = din("brt", [128, 36])
    cst_d = din("cst", [128, 1024])
    out_d = C.dram("out", [SEG, D], F32, kind="ExternalOutput")

    wA_bf = C.dram("wA_bf", [D, NA], BF16)
    wB_bf = C.dram("wB_bf", [D, 7168], BF16)
    wmem_bf = C.dram("wmem_bf", [D, 2048], BF16)
    wbr_bf = C.dram("wbr_bf", [4096, D], BF16)
    wout_bf = C.dram("wout_bf", [D, D], BF16)
    wBt = wB_bf
    wmkt = wmem_bf
    wmvt = wmem_bf
    wbrt = wbr_bf
    woutt = wout_bf

    def v_wB(cc0, ncc):
        return wB_bf.ap[:, cc0 * 128:(cc0 + ncc) * 128].rearrange("(c p) (a n) -> p a c n", p=128, a=ncc)

    def v_wB1(cc):
        return wB_bf.ap[:, cc * 128:(cc + 1) * 128].rearrange("(c p) n -> p c n", p=128)

    def v_wmk(oc):
        return wmem_bf.ap[:, oc * 128:(oc + 1) * 128].rearrange("(c p) n -> p c n", p=128)

    def v_wmv(ct):
        return wmem_bf.ap[:, 1024 + ct * 512:1024 + (ct + 1) * 512].rearrange("(c p) n -> p c n", p=128)

    def v_wbr(c, k0, nk):
        return wbr_bf.ap[k0 * 128:(k0 + nk) * 128, c * 128:(c + 1) * 128].rearrange("(c p) n -> p c n", p=128)

    def v_wout(ct):
        return wout_bf.ap[:, ct * 512:(ct + 1) * 512].rearrange("(c p) n -> p c n", p=128)
    if full:
        weg_bf = C.dram("weg_bf", [NEXP, D, DEXP], BF16)
        weu_bf = C.dram("weu_bf", [NEXP, D, DEXP], BF16)
        wed_bf = C.dram("wed_bf", [NEXP, DEXP, D], BF16)
        weg_loc = C.dram("weg_loc", [8, D * DEXP], BF16)
        weu_loc = C.dram("weu_loc", [8, D * DEXP], BF16)
        wed_loc = C.dram("wed_loc", [8, D * DEXP], BF16)
    qT_s = C.dram("qT_s", [4, 128, S], BF16)
    kT_s = C.dram("kT_s", [4, 128, S], BF16)
    v_s = C.dram("v_s", [2, S, 256], BF16)
    mq_s = C.dram("mq_s", [2, 128, S], BF16)
    mk_s = C.dram("mk_s", [2, 128, S], BF16)
    mv_s = C.dram("mv_s", [S, 256], BF16)
    mo_s = C.dram("mo_s", [S, 256], F32)
    gt_s = C.dram("gt_s", [2, 128, S], F32)
    NXC = S // 256
    exi = C.dram("exi", [NXC, 768, 256], BF16)
    exo = C.dram("exo", [NXC, 4 * 768, 256], BF16)

    for r0 in range(0, D, 128):
        dma("pool", wA_bf.ap[r0:r0 + 128], wA_d.ap[r0:r0 + 128], reads=[wA_d], writes=[wA_bf], multi=True)

    def cast_chunked(srcb, dstb, R, col0, ncc, nw):
        grp = max(1, 16 * 128 // nw // 1)
        grp = min(ncc, max(1, 2048 // 128))
        for kc in range(R // 128):
            for c0 in range(0, ncc, grp):
                g_ = min(grp, ncc - c0)
                s_ = srcb.ap[kc * 128:(kc + 1) * 128, col0 + c0 * nw:col0 + (c0 + g_) * nw].rearrange("p (cc n) -> p cc n", n=nw)
                d_ = dstb.ap[c0:c0 + g_, :, kc, :].rearrange("cc p n -> p cc n")
                dma("pool", d_, s_, reads=[srcb], writes=[dstb], multi=True)

    ccs = []

    def other_casts():
        for (srcb, dstb) in ((wB_d, wB_bf), (wmem_d, wmem_bf), (wbr_d, wbr_bf), (wout_d, wout_bf)):
            ncol = srcb.ap.shape[1]
            for r0 in range(0, srcb.ap.shape[0], 128):
                for c0 in range(0, ncol, 2048):
                    c1 = min(ncol, c0 + 2048)
                    dma("pool", dstb.ap[r0:r0 + 128, c0:c1], srcb.ap[r0:r0 + 128, c0:c1], reads=[srcb], writes=[dstb], multi=True)
        for (srcd, bfb) in ((weg_d, weg_bf), (weu_d, weu_bf), (wed_d, wed_bf)):
            nr = srcd.ap.shape[1]
            for e in range(NEXP):
                for r0 in range(0, nr, 128):
                    r1 = min(nr, r0 + 128)
                    dma("pool", bfb.ap[e, r0:r1, :], srcd.ap[e, r0:r1, :], reads=[srcd], writes=[bfb], multi=True)

    cst = C.sb("cst", [128, 1024], F32)
    dma("sp", cst.ap[:], cst_d.ap, reads=[cst_d], writes=[cst])
    identb = C.sb("identb", [128, 128], BF16)
    identf = C.sb("identf", [128, 128], F32)
    trib = C.sb("trib", [128, 128], BF16)
    maskneg = C.sb("maskneg", [128, 128], F32)
    alibi = C.sb("alibi", [128, 2, 130], F32)
    op("dve", lambda: nc.vector.tensor_copy(out=identb.ap[:], in_=cst.ap[:, 0:128]), [cst], [identb])
    op("dve", lambda: nc.vector.tensor_copy(out=identf.ap[:], in_=cst.ap[:, 0:128]), [cst], [identf])
    op("dve", lambda: nc.vector.tensor_copy(out=trib.ap[:], in_=cst.ap[:, 128:256]), [cst], [trib])
    op("dve", lambda: nc.vector.tensor_copy(out=maskneg.ap[:], in_=cst.ap[:, 256:384]), [cst], [maskneg])
    op("dve", lambda: nc.vector.tensor_copy(out=alibi.ap[:].rearrange("p a b -> p (a b)"), in_=cst.ap[:, 384:644]),
       [cst], [alibi])
    mkA1 = None
    hb = C.sb("hb", [128, 768], F32)
    dma("sp", hb.ap[:], hb_d.ap, reads=[hb_d], writes=[hb])
    fm = C.sb("fm", [128, 64], F32)
    dma("sp", fm.ap[:], fm_d.ap, reads=[fm_d], writes=[fm])
    lamt = C.sb("lamt", [128, 512], F32)
    dma("sp", lamt.ap[:], lam_d.ap, reads=[lam_d], writes=[lamt])
    brt = C.sb("brt", [128, 36], F32)
    dma("sp", brt.ap[:], brt_d.ap, reads=[brt_d], writes=[brt])
    sc = C.sb("sc", [128, 16], F32)
    lsc = C.sb("lsc", [128, 512], F32)
    op("dve", lambda: nc.vector.memset(sc.ap[:], 0.0), [], [sc])
    op("dve", lambda: nc.vector.memset(sc.ap[:, 2:3], EPS), [], [sc])
    op("dve", lambda: nc.vector.memset(sc.ap[:, 3:4], 1.0), [], [sc])
    op("dve", lambda: nc.vector.tensor_tensor(out=lsc.ap[:, 0:128], in0=lamt.ap[:, 0:128], in1=lamt.ap[:, 128:256], op=ALU.mult), [lamt], [lsc])
    op("dve", lambda: nc.vector.tensor_tensor(out=lsc.ap[:, 128:256], in0=lamt.ap[:, 256:384], in1=lamt.ap[:, 384:512], op=ALU.mult), [lamt, lsc], [lsc])
    op("dve", lambda: nc.vector.tensor_reduce(out=sc.ap[:, 8:10], in_=lsc.ap[:, 0:256].rearrange("p (a b) -> p a b", a=2), axis=AX.X, op=ALU.add), [lsc, sc], [sc])
    op("act", lambda: nc.scalar.activation(out=sc.ap[:, 10:12], in_=sc.ap[:, 8:10], func=AF.Exp), [sc], [sc])
    op("dve", lambda: nc.vector.tensor_tensor(out=sc.ap[:, 0:1], in0=sc.ap[:, 10:11], in1=sc.ap[:, 11:12], op=ALU.subtract), [sc], [sc])
    op("dve", lambda: nc.vector.tensor_scalar(out=sc.ap[:, 0:1], in0=sc.ap[:, 0:1], scalar1=LAM_INIT, scalar2=None, op0=ALU.add), [sc], [sc])
    op("dve", lambda: nc.vector.tensor_scalar(out=sc.ap[:, 1:2], in0=sc.ap[:, 0:1], scalar1=-1.0, scalar2=None, op0=ALU.mult), [sc], [sc])
    op("dve", lambda: nc.vector.tensor_scalar(out=sc.ap[:, 5:6], in0=cst.ap[:, 649:650], scalar1=-1.0, scalar2=None, op0=ALU.mult), [sc, cst], [sc])
    op("dve", lambda: nc.vector.tensor_copy(out=sc.ap[:, 6:7], in_=cst.ap[:, 648:649]), [sc, cst], [sc])

    import os
    KSTOP = os.environ.get("KSTOP", "")
    RET = lambda: (C.barrier(), (nc, C, {}))[1]
    if KSTOP == "const":
        return RET()
    PS = [C.ps(f"ps{i}", [128, 512], F32) for i in range(8)]

    def psbf(i):
        return PS[i].ap[:].bitcast(BF16)

    def rmsnorm_rows(xb, hout, gidx, tmp, stat, nb_cols=D, gb=None):
        op("act", lambda: nc.scalar.activation(out=tmp.ap[:], in_=xb.ap[:], func=AF.Square, accum_out=stat.ap[:, 0:1]), [xb], [tmp, stat])
        op("act", lambda: nc.scalar.activation(out=stat.ap[:, 1:2], in_=stat.ap[:, 0:1], func=AF.Ln, scale=1.0 / nb_cols, bias=sc.ap[:, 2:3]), [stat, sc], [stat])
        op("act", lambda: nc.scalar.activation(out=stat.ap[:, 2:3], in_=stat.ap[:, 1:2], func=AF.Exp, scale=-0.5), [stat], [stat])
        op("dve", lambda: nc.vector.scalar_tensor_tensor(out=hout.ap[:], in0=xb.ap[:], scalar=stat.ap[:, 2:3], in1=gb.ap[:, gidx, :], op0=ALU.mult, op1=ALU.mult), [xb, stat, gb], [hout])

    mkA1 = C.mark()
    gb = C.sb("gbA", [128, 1, D], F32)
    dma("sp", gb.ap[:], gb_d.ap[:, 0:1, :], reads=[gb_d], writes=[gb])
    wA = C.sb("wA", [128, 16, NA], BF16)
    dma("sp", wA.ap[:], wA_bf.ap.rearrange("(c p) n -> p c n", p=128), reads=[wA_bf], writes=[wA])
    wrep = C.sb("wrep", [128, 16, 2, 128], BF16)
    for j in range(2):
        op("pool", lambda j=j: nc.gpsimd.tensor_copy(out=wrep.ap[:, :, j, :], in_=wA.ap[:, :, 2560 + j:2561 + j].to_broadcast([128, 16, 128])), [wA], [wrep])

    xbuf = [C.sb(f"xb{i}", [128, D], F32) for i in range(2)]
    sqt = C.sb("sqt", [128, D], BF16)
    hbuf = [C.sb(f"hb{i}", [128, D], BF16) for i in range(2)]
    hT = [C.sb(f"hT{i}", [128, 16, 512], BF16) for i in range(1)]
    stats = [C.sb(f"st{i}", [128, 4], F32) for i in range(4)]
    fo = [C.sb(f"fo{i}", [128, 512], BF16) for i in range(4)]
    to = [C.sb(f"to{i}", [128, 512], BF16) for i in range(2)]
    tov = [C.sb(f"tov{i}", [128, 256], BF16) for i in range(2)]
    tof = [C.sb(f"tof{i}", [128, 256], F32) for i in range(2)]
    gto = [C.sb(f"gto{i}", [128, 512], F32) for i in range(2)]
    cpre = [C.sb(f"cpre{i}", [128, 3 + 512], F32) for i in range(4)]
    cacc = [C.sb(f"cacc{i}", [128, 512], F32) for i in range(2)]
    for i in range(4):
        op("dve", lambda i=i: nc.vector.memset(cpre[i].ap[:], 0.0), [], [cpre[i]])

    xi = 0
    for t in range(NT):
        hTt = hT[0]
        for blk in range(4):
            r0 = t * 512 + blk * 128
            xb = xbuf[xi % 2]
            hbf = hbuf[xi % 2]
            st = stats[xi % 4]
            xi += 1
            dma("sp", xb.ap[:], x_d.ap[r0:r0 + 128, :], reads=[x_d], writes=[xb])
            rmsnorm_rows(xb, hbf, 0, sqt, st, gb=gb)
            for half in range(2):
                pb = PS[half]
                for c8 in range(8):
                    c = half * 8 + c8
                    op("pe", lambda c=c, c8=c8, half=half: nc.tensor.transpose(out=psbf(half)[:, c8 * 128:(c8 + 1) * 128], in_=hbf.ap[:, c * 128:(c + 1) * 128], identity=identb.ap[:]), [hbf, identb], [pb])
                src = psbf(half).rearrange("p (c t) -> p c t", c=8)
                dst = hTt.ap[:, half * 8:(half + 1) * 8, blk * 128:(blk + 1) * 128]
                if half == 0:
                    op("act", lambda src=src, dst=dst: nc.scalar.copy(out=dst, in_=src), [pb], [hTt])
                else:
                    op("dve", lambda src=src, dst=dst: nc.vector.tensor_copy(out=dst, in_=src), [pb], [hTt])
        if KSTOP == "xT":
            return RET()
        t0 = t * 512
        pi = 2
        for oc in range(8):
            pb = PS[2 + (oc % 2)]
            for kc in range(16):
                op("pe", lambda oc=oc, kc=kc, pb=pb: nc.tensor.matmul(pb.ap[:], lhsT=wA.ap[:, kc, oc * 128:(oc + 1) * 128], rhs=hTt.ap[:, kc, :], start=(kc == 0), stop=(kc == 15)), [wA, hTt], [pb])
            f = fo[oc % 4]
            if oc % 2 == 0:
                op("act", lambda f=f, pb=pb: nc.scalar.copy(out=f.ap[:], in_=pb.ap[:]), [pb], [f])
            else:
                op("dve", lambda f=f, pb=pb: nc.vector.tensor_copy(out=f.ap[:], in_=pb.ap[:]), [pb], [f])
            dst = (qT_s if oc < 4 else kT_s)
            dma("sp", dst.ap[oc % 4, :, t0:t0 + 512], f.ap[:], reads=[f], writes=[dst], multi=True, owner=f)
        if KSTOP == "qk":
            return RET()
        for j in range(4):
            pb = PS[4 + (j % 2)]
            col0 = 1536 + j * 128
            for kc in range(16):
                op("pe", lambda kc=kc, pb=pb, col0=col0: nc.tensor.matmul(pb.ap[:], lhsT=wA.ap[:, kc, col0:col0 + 128], rhs=hTt.ap[:, kc, :], start=(kc == 0), stop=(kc == 15)), [wA, hTt], [pb])
            cp = cpre[j]
            ca = cacc[j % 2]
            op("act", lambda cp=cp, pb=pb: nc.scalar.copy(out=cp.ap[:, 3:515], in_=pb.ap[:]), [pb], [cp])
            wcol = 48 + j * 4
            op("dve", lambda cp=cp, ca=ca, wcol=wcol, j=j: nc.vector.tensor_scalar(out=ca.ap[:], in0=cp.ap[:, 0:512], scalar1=fm.ap[:, wcol:wcol + 1], scalar2=cst.ap[:, 644 + j:645 + j], op0=ALU.mult, op1=ALU.add), [cp, fm, cst], [ca])
            for tap in range(1, 4):
                op("dve", lambda cp=cp, ca=ca, wcol=wcol, tap=tap: nc.vector.scalar_tensor_tensor(out=ca.ap[:], in0=cp.ap[:, tap:tap + 512], scalar=fm.ap[:, wcol + tap:wcol + tap + 1], in1=ca.ap[:], op0=ALU.mult, op1=ALU.add), [cp, fm, ca], [ca])
            op("pool", lambda cp=cp: nc.gpsimd.tensor_copy(out=cp.ap[:, 0:3], in_=cp.ap[:, 512:515]), [cp], [cp])
            f = fo[j]
            op("act", lambda f=f, ca=ca, j=j: nc.scalar.activation(out=f.ap[:], in_=ca.ap[:], func=AF.Silu), [ca], [f])
            if j >= 2:
                op("pool", lambda f=f: nc.gpsimd.tensor_scalar(out=f.ap[:], in0=f.ap[:], scalar1=1.0 / 16.0, scalar2=None, op0=ALU.mult), [f], [f])
            dst = (mq_s if j < 2 else mk_s)
            dma("sp", dst.ap[j % 2, :, t0:t0 + 512], f.ap[:], reads=[f], writes=[dst], multi=True, owner=f)
        if KSTOP == "conv":
            return RET()
        for j in range(2):
            pb = PS[6 + j]
            for kc in range(16):
                op("pe", lambda kc=kc, pb=pb, j=j: nc.tensor.matmul(pb.ap[:], lhsT=wrep.ap[:, kc, j, :], rhs=hTt.ap[:, kc, :], start=(kc == 0), stop=(kc == 15)), [wrep, hTt], [pb])
            g = gto[j]
            op("act", lambda g=g, pb=pb: nc.scalar.copy(out=g.ap[:], in_=pb.ap[:]), [pb], [g])
            dma("sp", gt_s.ap[j, :, t0:t0 + 512], g.ap[:], reads=[g], writes=[gt_s], multi=True, owner=g)
        if KSTOP == "gates":
            return RET()
        for blk in range(4):
            r0 = t0 + blk * 128
            pb = PS[2 + (blk % 2)]
            for kc in range(16):
                op("pe", lambda kc=kc, pb=pb, blk=blk: nc.tensor.matmul(pb.ap[:], lhsT=hTt.ap[:, kc, blk * 128:(blk + 1) * 128], rhs=wA.ap[:, kc, 1024:1536], start=(kc == 0), stop=(kc == 15)), [wA, hTt], [pb])
            tb = to[blk % 2]
            op("dve", lambda tb=tb, pb=pb: nc.vector.tensor_copy(out=tb.ap[:], in_=pb.ap[:]), [pb], [tb])
            for hh in range(2):
                dma("sp", v_s.ap[hh, r0:r0 + 128, :], tb.ap[:, hh * 256:(hh + 1) * 256], reads=[tb], writes=[v_s], multi=True, owner=tb)
            pb2 = PS[4 + (blk % 2)]
            for kc in range(16):
                op("pe", lambda kc=kc, pb2=pb2, blk=blk: nc.tensor.matmul(pb2.ap[:], lhsT=hTt.ap[:, kc, blk * 128:(blk + 1) * 128], rhs=wA.ap[:, kc, 2048:2560], start=(kc == 0), stop=(kc == 15)), [wA, hTt], [pb2])
            tv = tov[blk % 2]
            tf = tof[blk % 2]
            op("dve", lambda tv=tv, pb2=pb2: nc.vector.tensor_copy(out=tv.ap[:], in_=pb2.ap[:, 0:256]), [pb2], [tv])
            op("act", lambda tf=tf, pb2=pb2: nc.scalar.activation(out=tf.ap[:], in_=pb2.ap[:, 256:512], func=AF.Sigmoid), [pb2], [tf])
            dma("sp", mv_s.ap[r0:r0 + 128, :], tv.ap[:], reads=[tv], writes=[mv_s], multi=True, owner=tv)
            dma("sp", mo_s.ap[r0:r0 + 128, :], tf.ap[:], reads=[tf], writes=[mo_s], multi=True, owner=tf)
        if KSTOP == "tile0":
            return RET()

    if full:
        other_casts()
    C.release(mkA1)
    mk2 = C.mark()
    KT = C.sb("KT", [128, 2, S], BF16)
    VV = C.sb("VV", [128, NBLK, 257], BF16)
    op("pool", lambda: nc.gpsimd.memset(VV.ap[:, :, 256:257], 1.0), [], [VV])
    QT = [C.sb(f"QT{i}", [128, 2, 256], BF16) for i in range(3)]
    Pb = [C.sb(f"Pb{i}", [128, 2, 256], BF16) for i in range(3)]
    o1 = [C.sb(f"o1_{i}", [128, 256], F32) for i in range(2)]
    osq = C.sb("osq", [128, 256], BF16)
    dab = [C.sb(f"dab{i}", [128, 256], BF16) for i in range(2)]
    daT = [C.sb(f"daT{i}", [128, 2, 128], BF16) for i in range(2)]
    ast = [C.sb(f"ast{i}", [128, 8], F32) for i in range(4)]
    NQT = S // 256
    SCL = 128 ** -0.5
    tri3 = trib.ap[:].unsqueeze(1).to_broadcast([128, 2, 128])
    si = 0
    fi = 0
    qi = 0
    for hl in range(2):
        rep_h = 3 if hl == 0 else 7
        dma("sp", KT.ap[:, 0, :], kT_s.ap[hl * 2], reads=[kT_s], writes=[KT])
        dma("sp", KT.ap[:, 1, :], kT_s.ap[hl * 2 + 1], reads=[kT_s], writes=[KT], multi=True)
        for n0 in range(0, NBLK, 16):
            dma("sp", VV.ap[:, n0:n0 + 16, 0:256], v_s.ap[hl, n0 * 128:(n0 + 16) * 128, :].rearrange("(n p) d -> p n d", p=128), reads=[v_s], writes=[VV], multi=True)
        for t in range(NQT):
            kbs = kb_list(rep_h, t, NBLK)
            qt = QT[qi % 3]
            qi += 1
            dma("sp", qt.ap[:], qT_s.ap[hl * 2:hl * 2 + 2, :, t * 256:(t + 1) * 256].rearrange("m d q -> d m q"), reads=[qT_s], writes=[qt])
            acc = [[PS[2], PS[3]], [PS[4], PS[5]]]
            for kb in kbs:
                d = kb - 2 * t
                qlo = 128 if d == 1 else 0
                sbk = PS[si % 2]
                P = Pb[si % 3]
                si += 1
                for m in range(2):
                    op("pe", lambda m=m, sbk=sbk, kb=kb, qt=qt, qlo=qlo: nc.tensor.matmul(sbk.ap[:, m * 256 + qlo:(m + 1) * 256], lhsT=KT.ap[:, m, kb * 128:(kb + 1) * 128], rhs=qt.ap[:, m, qlo:256], start=True, stop=True), [KT, qt], [sbk])
                S3 = sbk.ap[:].rearrange("p (m q) -> p m q", m=2)
                op("act", lambda P=P, S3=S3, qlo=qlo, d=d, hl=hl: nc.scalar.activation(out=P.ap[:, :, qlo:256], in_=S3[:, :, qlo:256], func=AF.Exp, bias=alibi.ap[:, hl, d + 128:d + 129], scale=SCL), [sbk, alibi], [P])
                if d >= 0:
                    dq = 0 if d == 0 else 128
                    op("dve", lambda P=P, dq=dq: nc.vector.tensor_tensor(out=P.ap[:, :, dq:dq + 128], in0=P.ap[:, :, dq:dq + 128], in1=tri3, op=ALU.mult), [P, trib], [P])
                for m in range(2):
                    for qb in range(2):
                        if d == 1 and qb == 0:
                            continue
                        a = acc[m][qb]
                        is_first = (kb == kbs[0])
                        is_last = (d == 0) if qb == 0 else (kb == kbs[-1])
                        op("pe", lambda a=a, P=P, m=m, qb=qb, kb=kb, is_first=is_first, is_last=is_last: nc.tensor.matmul(a.ap[:, 0:257], lhsT=P.ap[:, m, qb * 128:(qb + 1) * 128], rhs=VV.ap[:, kb, :], start=is_first, stop=is_last), [P, VV], [a])
            for qb in range(2):
                a1, a2 = acc[0][qb], acc[1][qb]
                st_ = ast[fi % 4]
                o = o1[fi % 2]
                db = dab[fi % 2]
                dT = daT[fi % 2]
                pbT = PS[6 + fi % 2]
                fi += 1
                op("dve", lambda st_=st_, a1=a1: nc.vector.reciprocal(out=st_.ap[:, 0:1], in_=a1.ap[:, 256:257]), [a1], [st_])
                op("dve", lambda st_=st_, a2=a2: nc.vector.reciprocal(out=st_.ap[:, 1:2], in_=a2.ap[:, 256:257]), [a2, st_], [st_])
                op("dve", lambda st_=st_: nc.vector.tensor_tensor(out=st_.ap[:, 2:3], in0=st_.ap[:, 1:2], in1=sc.ap[:, 1:2], op=ALU.mult), [st_, sc], [st_])
                op("act", lambda o=o, a1=a1, st_=st_: nc.scalar.activation(out=o.ap[:], in_=a1.ap[:, 0:256], func=AF.Copy, scale=st_.ap[:, 0:1]), [a1, st_], [o])
                op("dve", lambda o=o, a2=a2, st_=st_: nc.vector.scalar_tensor_tensor(out=o.ap[:], in0=a2.ap[:, 0:256], scalar=st_.ap[:, 2:3], in1=o.ap[:], op0=ALU.mult, op1=ALU.add), [a2, st_, o], [o])
                op("act", lambda o=o, st_=st_: nc.scalar.activation(out=osq.ap[:], in_=o.ap[:], func=AF.Square, accum_out=st_.ap[:, 3:4]), [o, st_], [osq, st_])
                op("act", lambda st_=st_: nc.scalar.activation(out=st_.ap[:, 4:5], in_=st_.ap[:, 3:4], func=AF.Ln, scale=1.0 / 256, bias=sc.ap[:, 2:3]), [st_, sc], [st_])
                op("act", lambda st_=st_: nc.scalar.activation(out=st_.ap[:, 5:6], in_=st_.ap[:, 4:5], func=AF.Exp, scale=-0.5), [st_], [st_])
                op("dve", lambda st_=st_: nc.vector.tensor_scalar(out=st_.ap[:, 5:6], in0=st_.ap[:, 5:6], scalar1=(1.0 - LAM_INIT), scalar2=None, op0=ALU.mult), [st_], [st_])
                op("dve", lambda db=db, o=o, st_=st_, hl=hl: nc.vector.scalar_tensor_tensor(out=db.ap[:], in0=o.ap[:], scalar=st_.ap[:, 5:6], in1=hb.ap[:, hl * 256:(hl + 1) * 256], op0=ALU.mult, op1=ALU.mult), [o, st_, hb], [db])
                for c in range(2):
                    op("pe", lambda c=c, db=db, pbT=pbT: nc.tensor.transpose(out=pbT.ap[:].bitcast(BF16)[:, c * 128:(c + 1) * 128], in_=db.ap[:, c * 128:(c + 1) * 128], identity=identb.ap[:]), [db, identb], [pbT])
                op("act", lambda dT=dT, pbT=pbT: nc.scalar.copy(out=dT.ap[:].rearrange("p c t -> p (c t)"), in_=pbT.ap[:].bitcast(BF16)[:, 0:256]), [pbT], [dT])
                tok0 = t * 256 + qb * 128
                dma("sp", exi.ap[t, hl * 256:(hl + 1) * 256, qb * 128:(qb + 1) * 128].rearrange("(c p) t -> p c t", p=128), dT.ap[:], reads=[dT], writes=[exi], multi=True, owner=dT)
    if KSTOP == "A2":
        return RET()
    C.release(mk2)
    mk3 = C.mark()
    SEGL = min(S, 2048)
    NSEG = S // SEGL
    NCH = SEGL // 128
    onesr = C.sb("onesr", [128, SEGL], F32)
    op("pool", lambda: nc.gpsimd.memset(onesr.ap[:], 1.0), [], [onesr])
    igb = C.sb("igb", [128, SEGL], F32)
    fgb = C.sb("fgb", [128, SEGL], F32)
    Gb = C.sb("Gb", [128, SEGL], F32)
    ub = C.sb("ub", [128, SEGL], F32)
    mb_ = C.sb("mb_", [128, SEGL], F32)
    tmp3 = C.sb("tmp3", [128, SEGL], F32)
    Pext = C.sb("Pext", [128, SEGL + 1], F32)
    carry = C.sb("carry", [128, 2], F32)
    op("dve", lambda: nc.vector.memset(carry.ap[:], 0.0), [], [carry])
    ucol = C.sb("ucol", [128, NCH], F32)
    mcol = C.sb("mcol", [128, NCH], F32)
    emc = C.sb("emc", [128, NCH], F32)
    wacol = C.sb("wacol", [128, NCH], F32)
    dcol = C.sb("dcol", [128, NCH], F32)
    t16 = C.sb("t16", [128, NCH], F32)
    t16b = C.sb("t16b", [128, NCH], F32)
    Cst = C.sb("Cst", [128, 2, 257], F32)
    Cbf = C.sb("Cbf", [128, 2, 257], BF16)
    op("dve", lambda: nc.vector.memset(Cst.ap[:], 0.0), [], [Cst])
    op("dve", lambda: nc.vector.memset(Cbf.ap[:], 0.0), [], [Cbf])
    R2 = 2
    mqb = [C.sb(f"mqb{i}", [128, 2, 128], BF16) for i in range(R2)]
    mkb = [C.sb(f"mkb{i}", [128, 2, 128], BF16) for i in range(R2)]
    mvb = [C.sb(f"mvb{i}", [128, 257], BF16) for i in range(R2)]
    for i in range(R2):
        op("pool", lambda i=i: nc.gpsimd.memset(mvb[i].ap[:, 256:257], 1.0), [], [mvb[i]])
    mob = [C.sb(f"mob{i}", [128, 256], F32) for i in range(R2)]
    irb = [C.sb(f"irb{i}", [128, 128], F32) for i in range(R2)]
    qpb = [C.sb(f"qpb{i}", [128, 2, 128], BF16) for i in range(R2)]
    Ttb = [C.sb(f"Ttb{i}", [128, 128], F32) for i in range(R2)]
    Dtb = [C.sb(f"Dtb{i}", [128, 128], F32) for i in range(R2)]
    Wtb = [C.sb(f"Wtb{i}", [128, 128], BF16) for i in range(R2)]
    ktmb = [C.sb(f"ktmb{i}", [128, 256], BF16) for i in range(R2)]
    vpb = [C.sb(f"vpb{i}", [128, 257], BF16) for i in range(R2)]
    fsb = [C.sb(f"fsb{i}", [128, 8], F32) for i in range(R2)]
    hhb = [C.sb(f"hhb{i}", [128, 256], F32) for i in range(R2)]
    hsq = C.sb("hsq", [128, 256], BF16)
    hh2b = [C.sb(f"hh2b{i}", [128, 256], F32) for i in range(R2)]
    hmbb = [C.sb(f"hmbb{i}", [128, 256], BF16) for i in range(R2)]
    hmTb = [C.sb(f"hmTb{i}", [128, 2, 128], BF16) for i in range(R2)]
    id3 = identf.ap[:].unsqueeze(1).to_broadcast([128, NCH, 128])
    ci = 0
    for sg in range(NSEG):
        s0 = sg * SEGL
        dma("sp", igb.ap[:], gt_s.ap[0, :, s0:s0 + SEGL], reads=[gt_s], writes=[igb])
        dma("sp", fgb.ap[:], gt_s.ap[1, :, s0:s0 + SEGL], reads=[gt_s], writes=[fgb])
        op("act", lambda: nc.scalar.activation(out=fgb.ap[:], in_=fgb.ap[:], func=AF.Exp, scale=-1.0, bias=sc.ap[:, 5:6]), [fgb, sc], [fgb])
        op("act", lambda: nc.scalar.activation(out=fgb.ap[:], in_=fgb.ap[:], func=AF.Ln, bias=sc.ap[:, 3:4]), [fgb, sc], [fgb])
        op("dve", lambda: nc.vector.tensor_tensor_scan(out=Gb.ap[:], data0=onesr.ap[:], data1=fgb.ap[:], initial=carry.ap[:, 0:1], op0=ALU.mult, op1=ALU.add), [onesr, fgb, carry], [Gb])
        op("dve", lambda: nc.vector.scalar_tensor_tensor(out=ub.ap[:], in0=igb.ap[:], scalar=sc.ap[:, 6:7], in1=Gb.ap[:], op0=ALU.add, op1=ALU.add), [igb, sc, Gb], [ub])
        op("dve", lambda: nc.vector.tensor_copy(out=Pext.ap[:, 0:1], in_=carry.ap[:, 1:2]), [carry], [Pext])
        op("dve", lambda: nc.vector.tensor_tensor_scan(out=Pext.ap[:, 1:SEGL + 1], data0=onesr.ap[:], data1=ub.ap[:], initial=carry.ap[:, 1:2], op0=ALU.mult, op1=ALU.max), [onesr, ub, carry, Pext], [Pext])
        op("dve", lambda: nc.vector.tensor_copy(out=carry.ap[:, 0:1], in_=Gb.ap[:, SEGL - 1:SEGL]), [Gb, carry], [carry])
        op("dve", lambda: nc.vector.tensor_copy(out=carry.ap[:, 1:2], in_=Pext.ap[:, SEGL:SEGL + 1]), [Pext, carry], [carry])
        op("dve", lambda: nc.vector.tensor_tensor(out=mb_.ap[:], in0=Pext.ap[:, 1:SEGL + 1], in1=Gb.ap[:], op=ALU.subtract), [Pext, Gb], [mb_])
        for (srcb, dstc) in ((ub, ucol), (mb_, mcol)):
            op("dve", lambda srcb=srcb: nc.vector.tensor_tensor(out=tmp3.ap[:].rearrange("p (c t) -> p c t", t=128), in0=srcb.ap[:].rearrange("p (c t) -> p c t", t=128), in1=id3, op=ALU.mult), [srcb, identf], [tmp3])
            op("dve", lambda dstc=dstc: nc.vector.tensor_reduce(out=dstc.ap[:], in_=tmp3.ap[:].rearrange("p (c t) -> p c t", t=128), axis=AX.X, op=ALU.add), [tmp3], [dstc])
        op("act", lambda: nc.scalar.activation(out=emc.ap[:], in_=mcol.ap[:], func=AF.Exp, scale=-1.0), [mcol], [emc])
        op("dve", lambda: nc.vector.tensor_tensor(out=t16.ap[:], in0=ucol.ap[:], in1=Pext.ap[:, 128:SEGL + 1:128], op=ALU.subtract), [ucol, Pext], [t16])
        op("act", lambda: nc.scalar.activation(out=wacol.ap[:], in_=t16.ap[:], func=AF.Exp), [t16], [wacol])
        op("dve", lambda: nc.vector.tensor_tensor(out=t16b.ap[:], in0=Pext.ap[:, 0:SEGL:128], in1=Pext.ap[:, 128:SEGL + 1:128], op=ALU.subtract), [Pext], [t16b])
        op("act", lambda: nc.scalar.activation(out=dcol.ap[:], in_=t16b.ap[:], func=AF.Exp), [t16b], [dcol])
        for j in range(NCH):
            tk0 = s0 + j * 128
            r = ci % R2
            ci += 1
            mq, mk, mv, mo, ir, qp, Tt, Dt, Wt, ktm, vp, fs, hh, hh2, hmb, hmT = (mqb[r], mkb[r], mvb[r], mob[r], irb[r], qpb[r], Ttb[r], Dtb[r], Wtb[r], ktmb[r], vpb[r], fsb[r], hhb[r], hh2b[r], hmbb[r], hmTb[r])
            dma("sp", mq.ap[:], mq_s.ap[:, :, tk0:tk0 + 128].rearrange("c d t -> d c t"), reads=[mq_s], writes=[mq])
            dma("sp", mk.ap[:], mk_s.ap[:, :, tk0:tk0 + 128].rearrange("c d t -> d c t"), reads=[mk_s], writes=[mk])
            dma("sp", mv.ap[:, 0:256], mv_s.ap[tk0:tk0 + 128, :], reads=[mv_s], writes=[mv])
            dma("sp", mo.ap[:], mo_s.ap[tk0:tk0 + 128, :], reads=[mo_s], writes=[mo])
            Pch = Pext.ap[:, 1 + j * 128:1 + (j + 1) * 128]
            op("act", lambda ir=ir, Pch=Pch, j=j: nc.scalar.activation(out=ir.ap[:], in_=Pch, func=AF.Exp, scale=-1.0, bias=Pext.ap[:, j * 128:j * 128 + 1]), [Pext], [ir])
            op("dve", lambda qp=qp, mq=mq, ir=ir: nc.vector.tensor_tensor(out=qp.ap[:], in0=mq.ap[:], in1=ir.ap[:].unsqueeze(1).to_broadcast([128, 2, 128]), op=ALU.mult), [mq, ir], [qp])
            op("dve", lambda Tt=Tt, Pch=Pch: nc.vector.tensor_tensor(out=Tt.ap[:], in0=maskneg.ap[:], in1=Pch, op=ALU.subtract), [maskneg, Pext], [Tt])
            op("act", lambda Dt=Dt, Tt=Tt, j=j: nc.scalar.activation(out=Dt.ap[:], in_=Tt.ap[:], func=AF.Exp, bias=ucol.ap[:, j:j + 1]), [Tt, ucol], [Dt])
            for c in range(2):
                op("pe", lambda c=c, mk=mk, mq=mq: nc.tensor.matmul(PS[0].ap[:, 0:128], lhsT=mk.ap[:, c, :], rhs=mq.ap[:, c, :], start=(c == 0), stop=(c == 1)), [mk, mq], [PS[0]])
            op("dve", lambda Wt=Wt, Dt=Dt: nc.vector.tensor_tensor(out=Wt.ap[:], in0=PS[0].ap[:, 0:128], in1=Dt.ap[:], op=ALU.mult), [PS[0], Dt], [Wt])
            op("pe", lambda Wt=Wt, mv=mv: nc.tensor.matmul(PS[1].ap[:, 0:257], lhsT=Wt.ap[:], rhs=mv.ap[:, 0:257], start=True, stop=False), [Wt, mv], [PS[1]])
            for c in range(2):
                op("pe", lambda c=c, qp=qp: nc.tensor.matmul(PS[1].ap[:, 0:257], lhsT=qp.ap[:, c, :], rhs=Cbf.ap[:, c, :], start=False, stop=(c == 1)), [qp, Cbf], [PS[1]])
            for c in range(2):
                op("pe", lambda c=c, mk=mk: nc.tensor.transpose(out=psbf(2)[:, c * 128:(c + 1) * 128], in_=mk.ap[:, c, :], identity=identb.ap[:]), [mk, identb], [PS[2]])
            op("act", lambda ktm=ktm: nc.scalar.copy(out=ktm.ap[:], in_=psbf(2)[:, 0:256]), [PS[2]], [ktm])
            op("pool", lambda vp=vp, mv=mv, j=j: nc.gpsimd.tensor_scalar(out=vp.ap[:], in0=mv.ap[:], scalar1=wacol.ap[:, j:j + 1], scalar2=None, op0=ALU.mult), [mv, wacol], [vp])
            for c in range(2):
                op("pe", lambda c=c, ktm=ktm, vp=vp: nc.tensor.matmul(PS[3 + c].ap[:, 0:257], lhsT=ktm.ap[:, c * 128:(c + 1) * 128], rhs=vp.ap[:], start=True, stop=True), [ktm, vp], [PS[3 + c]])
            for c in range(2):
                op("dve", lambda c=c, j=j: nc.vector.scalar_tensor_tensor(out=Cst.ap[:, c, :], in0=Cst.ap[:, c, :], scalar=dcol.ap[:, j:j + 1], in1=PS[3 + c].ap[:, 0:257], op0=ALU.mult, op1=ALU.add), [dcol, PS[3 + c]], [Cst])
            op("act", lambda: nc.scalar.copy(out=Cbf.ap[:], in_=Cst.ap[:]), [Cst], [Cbf])
            op("act", lambda fs=fs: nc.scalar.activation(out=fs.ap[:, 0:1], in_=PS[1].ap[:, 256:257], func=AF.Abs), [PS[1]], [fs])
            op("dve", lambda fs=fs, j=j: nc.vector.tensor_tensor(out=fs.ap[:, 0:1], in0=fs.ap[:, 0:1], in1=emc.ap[:, j:j + 1], op=ALU.max), [fs, emc], [fs])
            op("dve", lambda fs=fs: nc.vector.reciprocal(out=fs.ap[:, 1:2], in_=fs.ap[:, 0:1]), [fs], [fs])
            op("act", lambda hh=hh, fs=fs: nc.scalar.activation(out=hh.ap[:], in_=PS[1].ap[:, 0:256], func=AF.Copy, scale=fs.ap[:, 1:2]), [PS[1], fs], [hh])
            op("act", lambda hh=hh, fs=fs: nc.scalar.activation(out=hsq.ap[:], in_=hh.ap[:], func=AF.Square, accum_out=fs.ap[:, 2:3]), [hh, fs], [hsq, fs])
            op("act", lambda fs=fs: nc.scalar.activation(out=fs.ap[:, 3:4], in_=fs.ap[:, 2:3], func=AF.Ln, scale=1.0 / 256, bias=sc.ap[:, 2:3]), [fs, sc], [fs])
            op("act", lambda fs=fs: nc.scalar.activation(out=fs.ap[:, 4:5], in_=fs.ap[:, 3:4], func=AF.Exp, scale=-0.5), [fs], [fs])
            op("dve", lambda hh2=hh2, hh=hh, fs=fs: nc.vector.scalar_tensor_tensor(out=hh2.ap[:], in0=hh.ap[:], scalar=fs.ap[:, 4:5], in1=hb.ap[:, 512:768], op0=ALU.mult, op1=ALU.mult), [hh, fs, hb], [hh2])
            op("dve", lambda hmb=hmb, hh2=hh2, mo=mo: nc.vector.tensor_tensor(out=hmb.ap[:], in0=hh2.ap[:], in1=mo.ap[:], op=ALU.mult), [hh2, mo], [hmb])
            for c in range(2):
                op("pe", lambda c=c, hmb=hmb: nc.tensor.transpose(out=psbf(5)[:, c * 128:(c + 1) * 128], in_=hmb.ap[:, c * 128:(c + 1) * 128], identity=identb.ap[:]), [hmb, identb], [PS[5]])
            op("act", lambda hmT=hmT: nc.scalar.copy(out=hmT.ap[:].rearrange("p c t -> p (c t)"), in_=psbf(5)[:, 0:256]), [PS[5]], [hmT])
            dma("sp", exi.ap[tk0 // 256, 512:768, (tk0 % 256):(tk0 % 256) + 128].rearrange("(c p) t -> p c t", p=128), hmT.ap[:], reads=[hmT], writes=[exi], multi=True, owner=hmT)
    if KSTOP == "A3":
        return RET()
    C.release(mk3)
    if dbg and "exi_dbg" in dbg:
        exi_dbg = C.dram("exi_dbg", [768, S], BF16, kind="ExternalOutput")
        for ch in range(NXC):
            for r0 in range(0, 768, 128):
                dma("sp", exi_dbg.ap[r0:r0 + 128, ch * 256:(ch + 1) * 256], exi.ap[ch, r0:r0 + 128, :], reads=[exi], writes=[exi_dbg], multi=True)
    if not full:
        return RET()
    xs_ = nc.semaphore("xsem").__enter__()
    for ch in range(NXC):
        nc.gpsimd.collective_compute("AllGather", ALU.bypass, replica_groups=[[0, 1, 2, 3], [4, 5, 6, 7]], ins=[exi.ap[ch]], outs=[exo.ap[ch]]).then_inc(xs_)
    for e in C.eng:
        C.eng[e].wait_ge(xs_, NXC)
    if KSTOP == "X":
        return RET()
    for cs in ccs:
        for e in C.eng:
            C.eng[e].wait_ge(cs, 1)
    TB = 512
    NTB = SEG // TB
    if dbg and "x1_dbg" in dbg:
        x1_dbg = C.dram("x1_dbg", [SEG, D], F32, kind="ExternalOutput")
        rw_dbg = C.dram("rw_dbg", [SEG, 32], F32, kind="ExternalOutput")
    xt = C.sb("xt", [128, 4, D], F32)
    h2T = C.sb("h2T", [128, 16, TB], BF16)
    mT = C.sb("mT", [128, 16, 256], BF16)
    mkT = C.sb("mkT", [128, 8, 256], BF16)
    mvv = C.sb("mvv", [128, 2, 4, 257], BF16)
    wr_sb = C.sb("wr_sb", [128, 16, 36], F32)
    rw = C.sb("rw", [128, 4, 32], F32)
    onesb = C.sb("onesb", [128, 128], BF16)
    bst = [C.sb(f"bst{i}", [128, 8], F32) for i in range(4)]
    op("pool", lambda: nc.gpsimd.memset(onesb.ap[:], 1.0), [], [onesb])
    op("pool", lambda: nc.gpsimd.memset(mvv.ap[:], 1.0), [], [mvv])
    dma("sp", wr_sb.ap[:], wr_d.ap.rearrange("(c p) n -> p c n", p=128), reads=[wr_d], writes=[wr_sb])
    bsi = [0]

    def nst():
        bsi[0] += 1
        return bst[bsi[0] % 4]

    def transposes_bf(hbf, dst3, col0, ident=identb):
        for half in range(2):
            pb = PS[half]
            for c8 in range(8):
                c = half * 8 + c8
                op("pe", lambda c=c, c8=c8, half=half: nc.tensor.transpose(out=psbf(half)[:, c8 * 128:(c8 + 1) * 128], in_=hbf.ap[:, c * 128:(c + 1) * 128], identity=ident.ap[:]), [hbf, ident], [pb])
            src = psbf(half).rearrange("p (c t) -> p c t", c=8)
            dst = dst3.ap[:, half * 8:(half + 1) * 8, col0:col0 + 128]
            if half == 0:
                op("act", lambda src=src, dst=dst: nc.scalar.copy(out=dst, in_=src), [pb], [dst3])
            else:
                op("dve", lambda src=src, dst=dst: nc.vector.tensor_copy(out=dst, in_=src), [pb], [dst3])

    mk0 = C.mark()
    gmem = C.sb("gmem", [128, 1, D], F32)
    dma("sp", gmem.ap[:], gb_d.ap[:, 3:4, :], reads=[gb_d], writes=[gmem])
    memx = [C.sb(f"memx{i}", [128, D], F32) for i in range(2)]
    memh = [C.sb(f"memh{i}", [128, D], BF16) for i in range(2)]
    sqj = C.sb("sqj", [128, D], BF16)
    memT = C.sb("memT", [128, 16, 256], BF16)
    wbuf = [C.sb(f"wbuf{i}", [128, 8192], BF16) for i in range(2)]
    for blk in range(2):
        dma("sp", memx[blk].ap[:], mem_d.ap[blk * 128:(blk + 1) * 128, :], reads=[mem_d], writes=[memx[blk]])
        rmsnorm_rows(memx[blk], memh[blk], 0, sqj, nst(), gb=gmem)
        transposes_bf(memh[blk], memT, blk * 128)
    wi = 0
    for oc in range(8):
        wb = wbuf[wi % 2]
        wi += 1
        wb3 = wb.ap[:, 0:2048].rearrange("p (c n) -> p c n", n=128)
        dma("sp", wb3, v_wmk(oc), reads=[wmkt], writes=[wb])
        pb = PS[2 + oc % 2]
        for kc in range(16):
            op("pe", lambda kc=kc, pb=pb, wb3=wb3: nc.tensor.matmul(pb.ap[:, 0:256], lhsT=wb3[:, kc, :], rhs=memT.ap[:, kc, :], start=(kc == 0), stop=(kc == 15)), [wb, memT], [pb])
        op("act", lambda pb=pb, oc=oc: nc.scalar.copy(out=mkT.ap[:, oc, :], in_=pb.ap[:, 0:256]), [pb], [mkT])
    for ct in range(2):
        wb = wbuf[wi % 2]
        wi += 1
        wb3 = wb.ap[:].rearrange("p (c n) -> p c n", n=512)
        dma("sp", wb3, v_wmv(ct), reads=[wmvt], writes=[wb])
        for blk in range(2):
            pb = PS[4 + blk]
            for kc in range(16):
                op("pe", lambda kc=kc, pb=pb, wb3=wb3, blk=blk: nc.tensor.matmul(pb.ap[:], lhsT=memT.ap[:, kc, blk * 128:(blk + 1) * 128], rhs=wb3[:, kc, :], start=(kc == 0), stop=(kc == 15)), [wb, memT], [pb])
            op("dve", lambda pb=pb, blk=blk, ct=ct: nc.vector.tensor_copy(out=mvv.ap[:, blk, ct * 2:(ct + 1) * 2, 0:256], in_=pb.ap[:].rearrange("p (h d) -> p h d", h=2)), [pb], [mvv])
    C.release(mk0)
    if KSTOP == "B0":
        return RET()

    ohc = cst.ap[:, 652:656]
    for tb in range(NTB):
        tokb = tb * TB
        for blk in range(4):
            dma("sp", xt.ap[:, blk, :], xseg_d.ap[tokb + blk * 128:tokb + (blk + 1) * 128, :], reads=[xseg_d], writes=[xt], multi=(blk > 0))
        for hf in range(2):
            tok0 = tokb + hf * 256
            mk1 = C.mark()
            gmix = C.sb("gmix", [128, 1, D], F32)
            dma("sp", gmix.ap[:], gb_d.ap[:, 0:1, :], reads=[gb_d], writes=[gmix])
            hbb = [C.sb(f"hbb{i}", [128, D], BF16) for i in range(2)]
            sqj = C.sb("sqj1", [128, D], BF16)
            hT = C.sb("hTb", [128, 16, 256], BF16)
            exs = C.sb("exs", [128, 24, 256], BF16)
            cand = [C.sb(f"cand{i}", [128, 3, 256], BF16) for i in range(4)]
            xaq = C.sb("xaq", [128, 2, 256], BF16)
            Pm = C.sb("Pm", [128, 2, 256], BF16)
            xaT = C.sb("xaT", [128, 8, 256], BF16)
            rcp = C.sb("rcp", [128, 256], F32)
            wbuf = [C.sb(f"wbufa{i}", [128, 8192], BF16) for i in range(2)]
            sig = [C.sb(f"sig{i}", [128, 256], F32) for i in range(3)]
            tm = [C.sb(f"tm{i}", [128, 256], F32) for i in range(2)]
            for b2 in range(2):
                blk = hf * 2 + b2
                xv = Buf(xt.ap[:, blk, :], "xv")
                st = nst()
                op("act", lambda xv=xv, st=st: nc.scalar.activation(out=sqj.ap[:], in_=xv.ap, func=AF.Square, accum_out=st.ap[:, 0:1]), [xt, st], [sqj, st])
                op("act", lambda st=st: nc.scalar.activation(out=st.ap[:, 1:2], in_=st.ap[:, 0:1], func=AF.Ln, scale=1.0 / D, bias=sc.ap[:, 2:3]), [st, sc], [st])
                op("act", lambda st=st: nc.scalar.activation(out=st.ap[:, 2:3], in_=st.ap[:, 1:2], func=AF.Exp, scale=-0.5), [st], [st])
                op("dve", lambda xv=xv, st=st, b2=b2: nc.vector.scalar_tensor_tensor(out=hbb[b2].ap[:], in0=xv.ap, scalar=st.ap[:, 2:3], in1=gmix.ap[:, 0, :], op0=ALU.mult, op1=ALU.mult), [xt, st, gmix], [hbb[b2]])
                transposes_bf(hbb[b2], hT, b2 * 128)
            for pc in range(8):
                for s4 in range(4):
                    cd = cand[s4]
                    dma("sp", cd.ap[:], exo.ap[(s4 * SEG + tok0) // 256, pc * 384:(pc + 1) * 384, :].rearrange("(c p) t -> p c t", p=128), reads=[exo], writes=[cd])
                    dst = exs.ap[:, pc * 3:(pc + 1) * 3, :]
                    if s4 == 0:
                        op("dve", lambda cd=cd, dst=dst: nc.vector.tensor_scalar(out=dst, in0=cd.ap[:], scalar1=ohc[:, 0:1], scalar2=None, op0=ALU.mult), [cd, cst], [exs])
                    else:
                        op("dve", lambda cd=cd, dst=dst, s4=s4: nc.vector.scalar_tensor_tensor(out=dst, in0=cd.ap[:], scalar=ohc[:, s4:s4 + 1], in1=dst, op0=ALU.mult, op1=ALU.add), [cd, cst, exs], [exs])
            wi = 0
            for h in range(4):
                wb = wbuf[wi % 2]
                wi += 1
                wb4 = wb.ap[:, 0:4096].rearrange("p (a c n) -> p a c n", a=2, n=128)
                dma("sp", wb4[:, 0], v_wB1(2 * h), reads=[wBt], writes=[wb])
                dma("sp", wb4[:, 1], v_wB1(2 * h + 1), reads=[wBt], writes=[wb], multi=True)
                for c in range(2):
                    pb = PS[2 + c]
                    for kc in range(16):
                        op("pe", lambda kc=kc, pb=pb, wb4=wb4, c=c: nc.tensor.matmul(pb.ap[:, 0:256], lhsT=wb4[:, c, kc, :], rhs=hT.ap[:, kc, :], start=(kc == 0), stop=(kc == 15)), [wb, hT], [pb])
                    op("act", lambda pb=pb, c=c: nc.scalar.copy(out=xaq.ap[:, c, :], in_=pb.ap[:, 0:256]), [pb], [xaq])
                for mb in range(2):
                    pb = PS[4 + mb]
                    for c in range(2):
                        op("pe", lambda c=c, pb=pb, mb=mb, h=h: nc.tensor.matmul(pb.ap[:, 0:256], lhsT=mkT.ap[:, h * 2 + c, mb * 128:(mb + 1) * 128], rhs=xaq.ap[:, c, :], start=(c == 0), stop=(c == 1)), [mkT, xaq], [pb])
                    op("act", lambda pb=pb, mb=mb: nc.scalar.activation(out=Pm.ap[:, mb, :], in_=pb.ap[:, 0:256], func=AF.Exp, scale=1.0 / 16.0), [pb], [Pm])
                pbs = PS[6]
                for mb in range(2):
                    op("pe", lambda mb=mb: nc.tensor.matmul(pbs.ap[:, 0:256], lhsT=onesb.ap[:], rhs=Pm.ap[:, mb, :], start=(mb == 0), stop=(mb == 1)), [onesb, Pm], [pbs])
                op("dve", lambda: nc.vector.reciprocal(out=rcp.ap[:], in_=pbs.ap[:, 0:256]), [pbs], [rcp])
                for dvc in range(2):
                    pb = PS[2 + dvc]
                    for mb in range(2):
                        op("pe", lambda mb=mb, pb=pb, dvc=dvc, h=h: nc.tensor.matmul(pb.ap[:, 0:256], lhsT=mvv.ap[:, mb, h, dvc * 128:(dvc + 1) * 128], rhs=Pm.ap[:, mb, :], start=(mb == 0), stop=(mb == 1)), [mvv, Pm], [pb])
                    op("dve", lambda pb=pb, dvc=dvc, h=h: nc.vector.tensor_tensor(out=xaT.ap[:, h * 2 + dvc, :], in0=pb.ap[:, 0:256], in1=rcp.ap[:], op=ALU.mult), [pb, rcp], [xaT])
            for c in range(16):
                wg_ = wbuf[wi % 2]
                wi += 1
                wg4 = wg_.ap[:, 0:6144].rearrange("p (a c n) -> p a c n", a=3, n=128)
                for br in range(3):
                    dma("sp", wg4[:, br], v_wB1(8 + br * 16 + c), reads=[wBt], writes=[wg_], multi=(br > 0))
                wb_ = wbuf[wi % 2]
                wi += 1
                wb3 = wb_.ap[:, 0:4096].rearrange("p (c n) -> p c n", n=128)
                dma("sp", wb3[:, 0:16, :], v_wbr(c, 0, 16), reads=[wbrt], writes=[wb_])
                dma("sp", wb3[:, 16:32, :], v_wbr(c, 16, 16), reads=[wbrt], writes=[wb_], multi=True)
                for br in range(3):
                    pb = PS[br]
                    for kc in range(16):
                        op("pe", lambda kc=kc, pb=pb, br=br, wg4=wg4: nc.tensor.matmul(pb.ap[:, 0:256], lhsT=wg4[:, br, kc, :], rhs=hT.ap[:, kc, :], start=(kc == 0), stop=(kc == 15)), [wg_, hT], [pb])
                    op("act", lambda pb=pb, br=br, c=c: nc.scalar.activation(out=sig[br].ap[:], in_=pb.ap[:, 0:256], func=AF.Sigmoid, bias=fm.ap[:, br * 16 + c:br * 16 + c + 1]), [pb, fm], [sig[br]])
                lst = [(r * 4 + j, r * 6 + j) for r in range(4) for j in range(4)]
                for i_, (wk, ek) in enumerate(lst):
                    op("pe", lambda wk=wk, ek=ek, i_=i_, wb3=wb3: nc.tensor.matmul(PS[3].ap[:, 0:256], lhsT=wb3[:, wk, :], rhs=exs.ap[:, ek, :], start=(i_ == 0), stop=(i_ == 15)), [wb_, exs], [PS[3]])
                lst = [(16 + r * 2 + j, r * 6 + 4 + j) for r in range(4) for j in range(2)]
                for i_, (wk, ek) in enumerate(lst):
                    op("pe", lambda wk=wk, ek=ek, i_=i_, wb3=wb3: nc.tensor.matmul(PS[4].ap[:, 0:256], lhsT=wb3[:, wk, :], rhs=exs.ap[:, ek, :], start=(i_ == 0), stop=(i_ == 7)), [wb_, exs], [PS[4]])
                for kc in range(8):
                    op("pe", lambda kc=kc, wb3=wb3: nc.tensor.matmul(PS[5].ap[:, 0:256], lhsT=wb3[:, 24 + kc, :], rhs=xaT.ap[:, kc, :], start=(kc == 0), stop=(kc == 7)), [wb_, xaT], [PS[5]])
                op("dve", lambda: nc.vector.tensor_tensor(out=tm[0].ap[:], in0=PS[3].ap[:, 0:256], in1=sig[0].ap[:], op=ALU.mult), [PS[3], sig[0]], [tm[0]])
                op("dve", lambda: nc.vector.tensor_tensor(out=tm[1].ap[:], in0=PS[4].ap[:, 0:256], in1=sig[1].ap[:], op=ALU.mult), [PS[4], sig[1]], [tm[1]])
                op("pool", lambda: nc.gpsimd.tensor_tensor(out=tm[0].ap[:], in0=tm[0].ap[:], in1=tm[1].ap[:], op=ALU.add), [tm[0], tm[1]], [tm[0]])
                op("dve", lambda: nc.vector.tensor_tensor(out=tm[1].ap[:], in0=PS[5].ap[:, 0:256], in1=sig[2].ap[:], op=ALU.mult), [PS[5], sig[2]], [tm[1]])
                op("dve", lambda c=c: nc.vector.tensor_tensor(out=mT.ap[:, c, :], in0=tm[0].ap[:], in1=tm[1].ap[:], op=ALU.add), [tm[0], tm[1]], [mT])
            C.release(mk1)
            if KSTOP == "B1a":
                return RET()
            mk1 = C.mark()
            gffn = C.sb("gffn", [128, 1, D], F32)
            dma("sp", gffn.ap[:], gb_d.ap[:, 1:2, :], reads=[gb_d], writes=[gffn])
            wbuf = [C.sb(f"wbufb{i}", [128, 8192], BF16) for i in range(2)]
            h2f = C.sb("h2f", [128, D], F32)
            sqj = C.sb("sqj2", [128, D], BF16)
            h2Tf = C.sb("h2Tf", [128, 16, 128], F32)
            lg = C.sb("lg", [128, 36], F32)
            rt = C.sb("rt", [128, 160], F32)
            for ct in range(4):
                wb = wbuf[ct % 2]
                wb3 = wb.ap[:].rearrange("p (c n) -> p c n", n=512)
                dma("sp", wb3, v_wout(ct), reads=[woutt], writes=[wb])
                for b2 in range(2):
                    blk = hf * 2 + b2
                    pb = PS[2 + b2]
                    for kc in range(16):
                        op("pe", lambda kc=kc, pb=pb, wb3=wb3, b2=b2: nc.tensor.matmul(pb.ap[:], lhsT=mT.ap[:, kc, b2 * 128:(b2 + 1) * 128], rhs=wb3[:, kc, :], start=(kc == 0), stop=(kc == 15)), [wb, mT], [pb])
                    op("dve", lambda pb=pb, blk=blk, ct=ct: nc.vector.tensor_tensor(out=xt.ap[:, blk, ct * 512:(ct + 1) * 512], in0=pb.ap[:], in1=xt.ap[:, blk, ct * 512:(ct + 1) * 512], op=ALU.add), [pb], [xt])
            for b2 in range(2):
                blk = hf * 2 + b2
                st = nst()
                op("act", lambda blk=blk, st=st: nc.scalar.activation(out=sqj.ap[:], in_=xt.ap[:, blk, :], func=AF.Square, accum_out=st.ap[:, 0:1]), [xt, st], [sqj, st])
                op("act", lambda st=st: nc.scalar.activation(out=st.ap[:, 1:2], in_=st.ap[:, 0:1], func=AF.Ln, scale=1.0 / D, bias=sc.ap[:, 2:3]), [st, sc], [st])
                op("act", lambda st=st: nc.scalar.activation(out=st.ap[:, 2:3], in_=st.ap[:, 1:2], func=AF.Exp, scale=-0.5), [st], [st])
                op("dve", lambda blk=blk, st=st: nc.vector.scalar_tensor_tensor(out=h2f.ap[:], in0=xt.ap[:, blk, :], scalar=st.ap[:, 2:3], in1=gffn.ap[:, 0, :], op0=ALU.mult, op1=ALU.mult), [xt, st, gffn], [h2f])
                for q4 in range(4):
                    pb = PS[4 + q4]
                    for c4 in range(4):
                        c = q4 * 4 + c4
                        op("pe", lambda c=c, c4=c4, pb=pb: nc.tensor.transpose(out=pb.ap[:, c4 * 128:(c4 + 1) * 128], in_=h2f.ap[:, c * 128:(c + 1) * 128], identity=identf.ap[:]), [h2f, identf], [pb])
                    src = pb.ap[:].rearrange("p (c t) -> p c t", c=4)
                    op("act", lambda src=src, q4=q4: nc.scalar.copy(out=h2Tf.ap[:, q4 * 4:(q4 + 1) * 4, :], in_=src), [pb], [h2Tf])
                    op("dve", lambda src=src, q4=q4, blk=blk: nc.vector.tensor_copy(out=h2T.ap[:, q4 * 4:(q4 + 1) * 4, blk * 128:(blk + 1) * 128], in_=src), [pb], [h2T])
                pbr = PS[2]
                for kc in range(16):
                    op("pe", lambda kc=kc: nc.tensor.matmul(pbr.ap[:, 0:36], lhsT=h2Tf.ap[:, kc, :], rhs=wr_sb.ap[:, kc, :], start=(kc == 0), stop=(kc == 15)), [h2Tf, wr_sb], [pbr])
                BIG = 1.0e4
                A = rt.ap
                V = nc.vector
                op("dve", lambda: V.tensor_tensor(out=lg.ap[:], in0=pbr.ap[:, 0:36], in1=brt.ap[:], op=ALU.add), [pbr, brt], [lg])
                seq = [
                    lambda: V.tensor_reduce(out=A[:, 0:1], in_=lg.ap[:, 0:4], axis=AX.X, op=ALU.max),
                    lambda: V.tensor_scalar(out=A[:, 1:2], in0=A[:, 0:1], scalar1=-1.0, scalar2=None, op0=ALU.mult),
                    lambda: V.tensor_scalar(out=A[:, 8:12], in0=lg.ap[:, 0:4], scalar1=A[:, 0:1], scalar2=None, op0=ALU.is_ge),
                    lambda: V.tensor_scalar(out=A[:, 12:16], in0=A[:, 8:12], scalar1=BIG, scalar2=-BIG, op0=ALU.mult, op1=ALU.add),
                ]
                for f_ in seq:
                    op("dve", f_, [lg, rt], [rt])
                op("act", lambda: nc.scalar.activation(out=A[:, 16:20], in_=lg.ap[:, 0:4], func=AF.Exp, bias=A[:, 1:2]), [lg, rt], [rt])
                seq = [
                    lambda: V.tensor_reduce(out=A[:, 2:3], in_=A[:, 16:20], axis=AX.X, op=ALU.add),
                    lambda: V.reciprocal(out=A[:, 3:4], in_=A[:, 2:3]),
                ]
                for g4 in range(4):
                    seq.append(lambda g4=g4: V.tensor_scalar(out=A[:, 32 + g4 * 8:40 + g4 * 8], in0=lg.ap[:, 4 + g4 * 8:12 + g4 * 8], scalar1=A[:, 12 + g4:13 + g4], scalar2=None, op0=ALU.add))
                seq += [
                    lambda: V.tensor_reduce(out=A[:, 4:5], in_=A[:, 32:64], axis=AX.X, op=ALU.max),
                    lambda: V.tensor_scalar(out=A[:, 64:96], in0=A[:, 32:64], scalar1=A[:, 4:5], scalar2=None, op0=ALU.is_ge),
                    lambda: V.scalar_tensor_tensor(out=A[:, 96:128], in0=A[:, 64:96], scalar=-BIG, in1=A[:, 32:64], op0=ALU.mult, op1=ALU.add),
                    lambda: V.tensor_reduce(out=A[:, 5:6], in_=A[:, 96:128], axis=AX.X, op=ALU.max),
                    lambda: V.tensor_scalar(out=A[:, 128:160], in0=A[:, 96:128], scalar1=A[:, 5:6], scalar2=None, op0=ALU.is_ge),
                    lambda: V.tensor_tensor(out=A[:, 6:7], in0=A[:, 5:6], in1=A[:, 4:5], op=ALU.subtract),
                ]
                for f_ in seq:
                    op("dve", f_, [lg, rt], [rt])
                op("act", lambda: nc.scalar.activation(out=A[:, 7:8], in_=A[:, 6:7], func=AF.Exp), [rt], [rt])
                seq = [
                    lambda: V.tensor_scalar(out=A[:, 20:21], in0=A[:, 7:8], scalar1=1.0, scalar2=None, op0=ALU.add),
                    lambda: V.reciprocal(out=A[:, 21:22], in_=A[:, 20:21]),
                    lambda: V.tensor_tensor(out=A[:, 22:23], in0=A[:, 21:22], in1=A[:, 3:4], op=ALU.mult),
                    lambda: V.tensor_tensor(out=A[:, 23:24], in0=A[:, 22:23], in1=A[:, 7:8], op=ALU.mult),
                ]
                for f_ in seq:
                    op("dve", f_, [rt], [rt])
                op("dve", lambda blk=blk: V.tensor_scalar(out=rw.ap[:, blk, :], in0=A[:, 64:96], scalar1=A[:, 22:23], scalar2=None, op0=ALU.mult), [rt], [rw])
                op("dve", lambda blk=blk: V.scalar_tensor_tensor(out=rw.ap[:, blk, :], in0=A[:, 128:160], scalar=A[:, 23:24], in1=rw.ap[:, blk, :], op0=ALU.mult, op1=ALU.add), [rt], [rw])
            if dbg and "x1_dbg" in dbg:
                for b2 in range(2):
                    blk = hf * 2 + b2
                    dma("sp", x1_dbg.ap[tokb + blk * 128:tokb + (blk + 1) * 128, :], xt.ap[:, blk, :], reads=[xt], writes=[x1_dbg], multi=True, owner=x1_dbg)
                    dma("sp", rw_dbg.ap[tokb + blk * 128:tokb + (blk + 1) * 128, :], rw.ap[:, blk, :], reads=[rw], writes=[rw_dbg], multi=True, owner=rw_dbg)
            C.release(mk1)
            if KSTOP == "B1b":
                return RET()
        mk1 = C.mark()
        gfin = C.sb("gfin", [128, 1, D], F32)
        dma("sp", gfin.ap[:], gb_d.ap[:, 2:3, :], reads=[gb_d], writes=[gfin])
        wgb = C.sb("wgb", [128, 16, DEXP], BF16)
        wub = C.sb("wub", [128, 16, DEXP], BF16)
        wdb = C.sb("wdb", [128, 6, D], BF16)
        actT = C.sb("actT", [128, 6, TB], BF16)
        sil = [C.sb(f"sil{i}", [128, TB], F32) for i in range(2)]
        sqj = C.sb("sqj3", [128, D], BF16)
        ob = [C.sb(f"ob{i}", [128, D], F32) for i in range(2)]
        psi = 0
        for e in range(NEXP):
            for k0 in (0, 8):
                dma("sp", wgb.ap[:, k0:k0 + 8, :], weg_bf.ap[e, k0 * 128:(k0 + 8) * 128, :].rearrange("(c p) n -> p c n", p=128), reads=[weg_bf], writes=[wgb], multi=(k0 > 0))
                dma("sp", wub.ap[:, k0:k0 + 8, :], weu_bf.ap[e, k0 * 128:(k0 + 8) * 128, :].rearrange("(c p) n -> p c n", p=128), reads=[weu_bf], writes=[wub], multi=(k0 > 0))
            dma("sp", wdb.ap[:, 0:5, :], wed_bf.ap[e, 0:640, :].rearrange("(c p) n -> p c n", p=128), reads=[wed_bf], writes=[wdb])
            dma("sp", wdb.ap[0:64, 5, :], wed_bf.ap[e, 640:704, :], reads=[wed_bf], writes=[wdb], multi=True)
            for dc in range(6):
                M = 128 if dc < 5 else 64
                pa = PS[(psi % 2) * 2]
                pu = PS[(psi % 2) * 2 + 1]
                sl = sil[psi % 2]
                psi += 1
                for kc in range(16):
                    op("pe", lambda kc=kc, pa=pa, dc=dc, M=M: nc.tensor.matmul(pa.ap[0:M, :], lhsT=wgb.ap[:, kc, dc * 128:dc * 128 + M], rhs=h2T.ap[:, kc, :], start=(kc == 0), stop=(kc == 15)), [wgb, h2T], [pa])
                for kc in range(16):
                    op("pe", lambda kc=kc, pu=pu, dc=dc, M=M: nc.tensor.matmul(pu.ap[0:M, :], lhsT=wub.ap[:, kc, dc * 128:dc * 128 + M], rhs=h2T.ap[:, kc, :], start=(kc == 0), stop=(kc == 15)), [wub, h2T], [pu])
                op("act", lambda pa=pa, sl=sl, M=M: nc.scalar.activation(out=sl.ap[0:M, :], in_=pa.ap[0:M, :], func=AF.Silu), [pa], [sl])
                op("dve", lambda pu=pu, sl=sl, M=M, dc=dc: nc.vector.tensor_tensor(out=actT.ap[0:M, dc, :], in0=pu.ap[0:M, :], in1=sl.ap[0:M, :], op=ALU.mult), [pu, sl], [actT])
            for blk in range(4):
                for ct in range(4):
                    pb = PS[4 + (psi % 4)]
                    psi += 1
                    for dc in range(6):
                        M = 128 if dc < 5 else 64
                        op("pe", lambda dc=dc, M=M, pb=pb, blk=blk, ct=ct: nc.tensor.matmul(pb.ap[:], lhsT=actT.ap[0:M, dc, blk * 128:(blk + 1) * 128], rhs=wdb.ap[0:M, dc, ct * 512:(ct + 1) * 512], start=(dc == 0), stop=(dc == 5)), [actT, wdb], [pb])
                    op("dve", lambda pb=pb, blk=blk, ct=ct, e=e: nc.vector.scalar_tensor_tensor(out=xt.ap[:, blk, ct * 512:(ct + 1) * 512], in0=pb.ap[:], scalar=rw.ap[:, blk, e:e + 1], in1=xt.ap[:, blk, ct * 512:(ct + 1) * 512], op0=ALU.mult, op1=ALU.add), [pb, rw], [xt])
        for blk in range(4):
            st = nst()
            o_ = ob[blk % 2]
            op("act", lambda blk=blk, st=st: nc.scalar.activation(out=sqj.ap[:], in_=xt.ap[:, blk, :], func=AF.Square, accum_out=st.ap[:, 0:1]), [xt, st], [sqj, st])
            op("act", lambda st=st: nc.scalar.activation(out=st.ap[:, 1:2], in_=st.ap[:, 0:1], func=AF.Ln, scale=1.0 / D, bias=sc.ap[:, 2:3]), [st, sc], [st])
            op("act", lambda st=st: nc.scalar.activation(out=st.ap[:, 2:3], in_=st.ap[:, 1:2], func=AF.Exp, scale=-0.5), [st], [st])
            op("dve", lambda blk=blk, st=st, o_=o_: nc.vector.scalar_tensor_tensor(out=o_.ap[:], in0=xt.ap[:, blk, :], scalar=st.ap[:, 2:3], in1=gfin.ap[:, 0, :], op0=ALU.mult, op1=ALU.mult), [xt, st, gfin], [o_])
            dma("sp", out_d.ap[tokb + blk * 128:tokb + (blk + 1) * 128, :], o_.ap[:], reads=[o_], writes=[out_d], multi=True, owner=o_)
        C.release(mk1)
    C.barrier()
    return nc, C, dict(qT_s=qT_s, kT_s=kT_s, v_s=v_s, mq_s=mq_s, mk_s=mk_s, mv_s=mv_s, mo_s=mo_s, gt_s=gt_s, exi=exi, exo=exo, out=out_d)


def _pack_inputs(inp, S):
    f32 = np.float32
    SEG = S // 4
    w_in = np.asarray(inp["w_in"][0], f32)
    x = np.asarray(inp["x"], f32)[:, :S]
    mem = np.asarray(inp["mem"], f32)
    wbd = np.asarray(inp["w_branch_diff"][0], f32)
    rows = []
    for r in range(4):
        for j in range(4):
            h = HEAD_PAIRS[r][j // 2]
            r0 = h * 256 + (j % 2) * 128
            rows.append(wbd[r0:r0 + 128])
    wbr = np.concatenate(rows + [np.asarray(inp["w_branch_mlstm"][0], f32), np.asarray(inp["w_branch_cross"][0], f32)], axis=0)
    wB = np.ascontiguousarray(w_in[:, 10248:17416])
    wr = np.concatenate([np.asarray(inp["w_router_group"][0], f32), np.asarray(inp["w_router_expert"][0], f32)], axis=1)
    gbv = np.stack([np.asarray(inp[k], f32).reshape(-1) for k in ("g_mix", "g_ffn", "g_final", "g_mem")], 0)
    gb = np.ascontiguousarray(np.broadcast_to(gbv[None], (128, 4, D)))
    lamv = np.concatenate([np.asarray(inp[k], f32).reshape(-1) for k in ("lam_q1", "lam_k1", "lam_q2", "lam_k2")])
    lam = np.ascontiguousarray(np.broadcast_to(lamv[None], (128, 512)))
    brv = np.concatenate([np.asarray(inp["b_router_group"], f32).reshape(-1), np.asarray(inp["b_router_expert"], f32).reshape(-1)])
    brt = np.ascontiguousarray(np.broadcast_to(brv[None], (128, 36)))
    conv_w = np.asarray(inp["conv_w"][0], f32)
    conv_b = np.asarray(inp["conv_b"][0], f32)
    b_gate = np.asarray(inp["b_gate"][0], f32)
    gdh = np.asarray(inp["g_diff_head"][0], f32)
    gmh = np.asarray(inp["g_mlstm_head"][0], f32)
    p = np.arange(128)
    maps = []
    for c in range(NCORES):
        b, g = c // 4, c % 4
        hs = HEAD_PAIRS[g]
        hb = np.concatenate([gdh[hs[0]], gdh[hs[1]], gmh[g]])
        hb = np.ascontiguousarray(np.broadcast_to(hb[None], (128, 768)))
        fm = np.zeros((128, 64), f32)
        fm[:, 0:48] = b_gate.reshape(48, 128).T
        for j in range(4):
            base = (g * 256 + j * 128) if j < 2 else (1024 + g * 256 + (j - 2) * 128)
            for tap in range(4):
                fm[:, 48 + j * 4 + tap] = conv_w[tap, base:base + 128]
        cst = np.zeros((128, 1024), f32)
        cst[:, 0:128] = np.eye(128, dtype=f32)
        cst[:, 128:256] = (p[:, None] <= p[None, :]).astype(f32)
        cst[:, 256:384] = np.where(p[:, None] <= p[None, :], 0.0, -30000.0).astype(f32)
        for hl in range(2):
            sl = SLOPES[hs[hl]]
            idx = np.arange(130)
            cst[:, 384 + hl * 130:384 + (hl + 1) * 130] = sl * (p[:, None] - 128 + 128 * (idx[None, :] - 128))
        for j in range(4):
            base = (g * 256 + j * 128) if j < 2 else (1024 + g * 256 + (j - 2) * 128)
            cst[:, 644 + j] = conv_b[base:base + 128]
        cst[:, 648] = np.asarray(inp["b_igate"], f32).reshape(-1)[g]
        cst[:, 649] = np.asarray(inp["b_fgate"], f32).reshape(-1)[g]
        cst[:, 652 + g] = 1.0
        maps.append({
            "x": np.ascontiguousarray(x[b]),
            "xseg": np.ascontiguousarray(x[b, g * SEG:(g + 1) * SEG]),
            "mem": np.ascontiguousarray(mem[b]),
            "wA": np.ascontiguousarray(w_in[:, head_cols(g)]),
            "wB": wB,
            "wmem": np.asarray(inp["w_mem_kv"][0], f32),
            "wbr": wbr,
            "wout": np.asarray(inp["w_out"][0], f32),
            "wr": wr,
            "weg": np.asarray(inp["w_expert_gate"][0], f32),
            "weu": np.asarray(inp["w_expert_up"][0], f32),
            "wed": np.asarray(inp["w_expert_down"][0], f32),
            "gb": gb, "hb": hb, "fm": fm, "lam": lam, "brt": brt, "cst": cst,
        })
    return maps


def run(inp, S, dbg=False, trace=False, full=True):
    nc, C, bufs = build(S, dbg=dbg, full=full)
    maps = _pack_inputs(inp, S)
    if not full:
        for m in maps:
            for k in ("weg", "weu", "wed"):
                m.pop(k)
    res = run_bass_kernel_spmd(nc, maps, core_ids=list(range(NCORES)), trace=trace)
    return res


def kernel(**inputs):
    S = 16384
    res = run(inputs, S)
    SEG = S // 4
    out = np.zeros((2, S, D), np.float32)
    for c in range(NCORES):
        b, g = c // 4, c % 4
        out[b, g * SEG:(g + 1) * SEG] = res.results[c]["out"]
    return out
```

```python
import numpy as np
import concourse.bass as bass
import concourse.mybir as mybir
from concourse.bass_utils import run_bass_kernel_spmd

F32 = mybir.dt.float32
BF16 = mybir.dt.bfloat16
I32 = mybir.dt.int32
AF = mybir.ActivationFunctionType
ALU = mybir.AluOpType
AX = mybir.AxisListType

D = 2048
NCORES = 8
EPS = 1e-6
DEXP = 704
NEXP = 32
LAM_INIT = 0.8 - 0.6 * 1.0
WIN_CUT = 100.0
SLOPES = [2.0 ** (-8.0 * (h + 1) / 8) for h in range(8)]
HEAD_PAIRS = [(0, 7), (1, 6), (2, 5), (3, 4)]


class Buf:
    def __init__(self, ap, name):
        self.ap = ap
        self.name = name
        self.last_w = []
        self.readers = []
        self.dsem = None
        self.dcount = 0
        self.psum = False

    def __getitem__(self, k):
        return self.ap[k]


class Ctx:
    SEM_LIMIT = 30000

    dbg_names = ()

    def __init__(self, nc):
        self.nc = nc
        self.eng = {"pe": nc.tensor, "act": nc.scalar, "dve": nc.vector, "pool": nc.gpsimd, "sp": nc.sync}
        self.sem = {}
        self.cnt = {}
        self.nsem = 0
        for e in self.eng:
            self._new_sem(e)
        self.waited = {e: {} for e in self.eng}
        self.stack = []
        self.allbufs = []
        self.sem_pool = []
        self.uid = 0
        self.tot = {}

    def _new_sem(self, e):
        s = self.nc.semaphore(f"s_{e}_{self.nsem}")
        self.nsem += 1
        self.sem[e] = s.__enter__()
        self.cnt[e] = 0

    def sb(self, name, shape, dt):
        self.uid += 1
        cm = self.nc.sbuf_tensor(f"sb_{name}_{self.uid}", shape, dt)
        t = cm.__enter__()
        b = Buf(t, name)
        self.stack.append((cm, b))
        self.allbufs.append(b)
        return b

    def mark(self):
        return len(self.stack)

    def barrier(self):
        evs = []
        for e in ("pe", "act", "dve", "pool"):
            if self.cnt[e] > 0:
                evs.append((self.sem[e], self.cnt[e], None))
        for b in self.allbufs:
            if b.dsem is not None and b.dcount > 0:
                evs.append((b.dsem, b.dcount, None))
        for e in self.eng:
            self._wait(e, evs)

    def release(self, mark):
        self.barrier()
        while len(self.stack) > mark:
            cm, b = self.stack.pop()
            cm.__exit__(None, None, None)
            if b.dsem is not None:
                if b.dcount < 20000:
                    self.sem_pool.append((b.dsem, b.dcount))
                b.dsem = None
            if b in self.allbufs:
                self.allbufs.remove(b)

    def ps(self, name, shape, dt):
        t = self.nc.psum_tensor("pp_" + name, shape, dt).__enter__()
        b = Buf(t, name)
        b.psum = True
        return b

    def dram(self, name, shape, dt, kind="Internal"):
        if kind == "Internal" and name in self.dbg_names:
            kind = "ExternalOutput"
        t = self.nc.dram_tensor(name, shape, dt, kind=kind)
        b = Buf(t.ap(), name)
        self.allbufs.append(b)
        return b

    def _wait(self, e, events):
        eng = self.eng[e]
        w = self.waited[e]
        for (sem, val, src) in events:
            if src == "pe" and e == "pe":
                continue
            key = id(sem)
            if w.get(key, (None, 0))[1] >= val:
                continue
            eng.wait_ge(sem, val)
            w[key] = (sem, val)

    def _deps(self, reads, writes):
        ev = []
        for b in reads:
            ev += b.last_w
        for b in writes:
            ev += b.last_w + b.readers
        return ev

    def op(self, e, fn, reads=(), writes=()):
        pr = [b for b in reads if b.psum and b not in writes]
        if pr:
            writes = list(writes) + pr
            reads = [b for b in reads if not b.psum]
        self._wait(e, self._deps(reads, writes))
        ins = fn()
        if self.cnt[e] >= self.SEM_LIMIT:
            self._new_sem(e)
        self.cnt[e] += 1
        self.tot[e] = self.tot.get(e, 0) + 1
        ins.then_inc(self.sem[e], 1)
        evt = (self.sem[e], self.cnt[e], e)
        for b in writes:
            b.last_w = [evt]
            b.readers = []
        for b in reads:
            if b in writes:
                continue
            b.readers = [x for x in b.readers if x[2] != e or x[0] is not evt[0]] + [evt]
        return ins

    def dma(self, q, out, in_, reads=(), writes=(), multi=False, owner=None, **kw):
        ev = []
        for b in reads:
            ev += b.last_w
        for b in writes:
            if multi:
                ev += b.readers
            else:
                ev += b.last_w + b.readers
        self._wait(q, ev)
        tgt = owner if owner is not None else writes[0]
        if tgt.dsem is None:
            if self.sem_pool:
                tgt.dsem, tgt.dcount = self.sem_pool.pop()
            else:
                s = self.nc.semaphore(f"d_{self.nsem}")
                self.nsem += 1
                tgt.dsem = s.__enter__()
        ins = self.eng[q].dma_start(out=out, in_=in_, **kw)
        tgt.dcount += 16
        ins.then_inc(tgt.dsem, 16)
        evt = (tgt.dsem, tgt.dcount, None)
        for b in writes:
            if multi:
                b.last_w = [x for x in b.last_w if x[0] is not tgt.dsem] + [evt]
            else:
                b.last_w = [evt]
                b.readers = []
        for b in reads:
            b.readers = [x for x in b.readers if x[0] is not tgt.dsem] + [evt]
        return ins

    def drain(self, e, bufs):
        ev = []
        for b in bufs:
            ev += b.last_w + b.readers
        self._wait(e, ev)


def head_cols(g):
    hs = HEAD_PAIRS[g]
    cols = []
    for h in hs:
        cols += list(range(h * 256, h * 256 + 256))
    for h in hs:
        cols += list(range(2048 + h * 256, 2048 + h * 256 + 256))
    for h in hs:
        cols += list(range(4096 + h * 256, 4096 + h * 256 + 256))
    cols += list(range(6144 + g * 256, 6144 + g * 256 + 256))
    cols += list(range(6144 + 1024 + g * 256, 6144 + 1024 + g * 256 + 256))
    cols += list(range(8192 + g * 256, 8192 + g * 256 + 256))
    cols += list(range(9216 + g * 256, 9216 + g * 256 + 256))
    cols += [10240 + g, 10240 + 4 + g]
    return np.array(cols)


NA = 2562


def kb_list(h, t, NBLK):
    slope = SLOPES[h]
    out = []
    for kb in range(0, 2 * t + 2):
        dmin = (2 * t) * 128 - (kb * 128 + 127)
        if dmin > 0 and slope * dmin > WIN_CUT:
            continue
        out.append(kb)
    return out


def build(S, dbg=False, full=True):
    nc = bass.Bass("TRN2", target_bir_lowering=False)
    C = Ctx(nc)
    if dbg:
        C.dbg_names = dbg
    op, dma = C.op, C.dma
    NT = S // 512
    NBLK = S // 128
    SEG = S // 4
    NTB = SEG // 512

    def din(name, shape, dt=F32):
        return C.dram(name, shape, dt, kind="ExternalInput")

    x_d = din("x", [S, D])
    xseg_d = din("xseg", [SEG, D])
    mem_d = din("mem", [256, D])
    wA_d = din("wA", [D, NA])
    wB_d = din("wB", [D, 7168])
    wmem_d = din("wmem", [D, 2048])
    wbr_d = din("wbr", [4096, D])
    wout_d = din("wout", [D, D])
    wr_d = din("wr", [D, 36])
    if full:
        weg_d = din("weg", [NEXP, D, DEXP])
        weu_d = din("weu", [NEXP, D, DEXP])
        wed_d = din("wed", [NEXP, DEXP, D])
    gb_d = din("gb", [128, 4, D])
    hb_d = din("hb", [128, 768])
    fm_d = din("fm", [128, 64])
    lam_d = din("lam", [128, 512])
    brt_d = din("brt", [128, 36])
    cst_d = din("cst", [128, 1024])
    out_d = C.dram("out", [SEG, D], F32, kind="ExternalOutput")

    wA_bf = C.dram("wA_bf", [D, NA], BF16)
    wB_bf = C.dram("wB_bf", [D, 7168], BF16)
    wmem_bf = C.dram("wmem_bf", [D, 2048], BF16)
    wbr_bf = C.dram("wbr_bf", [4096, D], BF16)
    wout_bf = C.dram("wout_bf", [D, D], BF16)
    wBt = wB_bf
    wmkt = wmem_bf
    wmvt = wmem_bf
    wbrt = wbr_bf
    woutt = wout_bf

    def v_wB(cc0, ncc):
        return wB_bf.ap[:, cc0 * 128:(cc0 + ncc) * 128].rearrange("(c p) (a n) -> p a c n", p=128, a=ncc)

    def v_wB1(cc):
        return wB_bf.ap[:, cc * 128:(cc + 1) * 128].rearrange("(c p) n -> p c n", p=128)

    def v_wmk(oc):
        return wmem_bf.ap[:, oc * 128:(oc + 1) * 128].rearrange("(c p) n -> p c n", p=128)

    def v_wmv(ct):
        return wmem_bf.ap[:, 1024 + ct * 512:1024 + (ct + 1) * 512].rearrange("(c p) n -> p c n", p=128)

    def v_wbr(c, k0, nk):
        return wbr_bf.ap[k0 * 128:(k0 + nk) * 128, c * 128:(c + 1) * 128].rearrange("(c p) n -> p c n", p=128)

    def v_wout(ct):
        return wout_bf.ap[:, ct * 512:(ct + 1) * 512].rearrange("(c p) n -> p c n", p=128)
    if full:
        weg_bf = C.dram("weg_bf", [NEXP, D, DEXP], BF16)
        weu_bf = C.dram("weu_bf", [NEXP, D, DEXP], BF16)
        wed_bf = C.dram("wed_bf", [NEXP, DEXP, D], BF16)
        weg_loc = C.dram("weg_loc", [8, D * DEXP], BF16)
        weu_loc = C.dram("weu_loc", [8, D * DEXP], BF16)
        wed_loc = C.dram("wed_loc", [8, D * DEXP], BF16)
    qT_s = C.dram("qT_s", [4, 128, S], BF16)
    kT_s = C.dram("kT_s", [4, 128, S], BF16)
    v_s = C.dram("v_s", [2, S, 256], BF16)
    mq_s = C.dram("mq_s", [2, 128, S], BF16)
    mk_s = C.dram("mk_s", [2, 128, S], BF16)
    mv_s = C.dram("mv_s", [S, 256], BF16)
    mo_s = C.dram("mo_s", [S, 256], F32)
    gt_s = C.dram("gt_s", [2, 128, S], F32)
    NXC = S // 256
    exi = C.dram("exi", [NXC, 768, 256], BF16)
    exo = C.dram("exo", [NXC, 4 * 768, 256], BF16)

    for r0 in range(0, D, 128):
        dma("pool", wA_bf.ap[r0:r0 + 128], wA_d.ap[r0:r0 + 128], reads=[wA_d], writes=[wA_bf], multi=True)

    def cast_chunked(srcb, dstb, R, col0, ncc, nw):
        grp = max(1, 16 * 128 // nw // 1)
        grp = min(ncc, max(1, 2048 // 128))
        for kc in range(R // 128):
            for c0 in range(0, ncc, grp):
                g_ = min(grp, ncc - c0)
                s_ = srcb.ap[kc * 128:(kc + 1) * 128, col0 + c0 * nw:col0 + (c0 + g_) * nw].rearrange("p (cc n) -> p cc n", n=nw)
                d_ = dstb.ap[c0:c0 + g_, :, kc, :].rearrange("cc p n -> p cc n")
                dma("pool", d_, s_, reads=[srcb], writes=[dstb], multi=True)

    ccs = []

    def other_casts():
        for (srcb, dstb) in ((wB_d, wB_bf), (wmem_d, wmem_bf), (wbr_d, wbr_bf), (wout_d, wout_bf)):
            ncol = srcb.ap.shape[1]
            for r0 in range(0, srcb.ap.shape[0], 128):
                for c0 in range(0, ncol, 2048):
                    c1 = min(ncol, c0 + 2048)
                    dma("pool", dstb.ap[r0:r0 + 128, c0:c1], srcb.ap[r0:r0 + 128, c0:c1], reads=[srcb], writes=[dstb], multi=True)
        for (srcd, bfb) in ((weg_d, weg_bf), (weu_d, weu_bf), (wed_d, wed_bf)):
            nr = srcd.ap.shape[1]
            for e in range(NEXP):
                for r0 in range(0, nr, 128):
                    r1 = min(nr, r0 + 128)
                    dma("pool", bfb.ap[e, r0:r1, :], srcd.ap[e, r0:r1, :], reads=[srcd], writes=[bfb], multi=True)

    cst = C.sb("cst", [128, 1024], F32)
    dma("sp", cst.ap[:], cst_d.ap, reads=[cst_d], writes=[cst])
    identb = C.sb("identb", [128, 128], BF16)
    identf = C.sb("identf", [128, 128], F32)
    trib = C.sb("trib", [128, 128], BF16)
    maskneg = C.sb("maskneg", [128, 128], F32)
    alibi = C.sb("alibi", [128, 2, 130], F32)
    op("dve", lambda: nc.vector.tensor_copy(out=identb.ap[:], in_=cst.ap[:, 0:128]), [cst], [identb])
    op("dve", lambda: nc.vector.tensor_copy(out=identf.ap[:], in_=cst.ap[:, 0:128]), [cst], [identf])
    op("dve", lambda: nc.vector.tensor_copy(out=trib.ap[:], in_=cst.ap[:, 128:256]), [cst], [trib])
    op("dve", lambda: nc.vector.tensor_copy(out=maskneg.ap[:], in_=cst.ap[:, 256:384]), [cst], [maskneg])
    op("dve", lambda: nc.vector.tensor_copy(out=alibi.ap[:].rearrange("p a b -> p (a b)"), in_=cst.ap[:, 384:644]),
       [cst], [alibi])
    mkA1 = None
    hb = C.sb("hb", [128, 768], F32)
    dma("sp", hb.ap[:], hb_d.ap, reads=[hb_d], writes=[hb])
    fm = C.sb("fm", [128, 64], F32)
    dma("sp", fm.ap[:], fm_d.ap, reads=[fm_d], writes=[fm])
    lamt = C.sb("lamt", [128, 512], F32)
    dma("sp", lamt.ap[:], lam_d.ap, reads=[lam_d], writes=[lamt])
    brt = C.sb("brt", [128, 36], F32)
    dma("sp", brt.ap[:], brt_d.ap, reads=[brt_d], writes=[brt])
    sc = C.sb("sc", [128, 16], F32)
    lsc = C.sb("lsc", [128, 512], F32)
    op("dve", lambda: nc.vector.memset(sc.ap[:], 0.0), [], [sc])
    op("dve", lambda: nc.vector.memset(sc.ap[:, 2:3], EPS), [], [sc])
    op("dve", lambda: nc.vector.memset(sc.ap[:, 3:4], 1.0), [], [sc])
    op("dve", lambda: nc.vector.tensor_tensor(out=lsc.ap[:, 0:128], in0=lamt.ap[:, 0:128], in1=lamt.ap[:, 128:256], op=ALU.mult), [lamt], [lsc])
    op("dve", lambda: nc.vector.tensor_tensor(out=lsc.ap[:, 128:256], in0=lamt.ap[:, 256:384], in1=lamt.ap[:, 384:512], op=ALU.mult), [lamt, lsc], [lsc])
    op("dve", lambda: nc.vector.tensor_reduce(out=sc.ap[:, 8:10], in_=lsc.ap[:, 0:256].rearrange("p (a b) -> p a b", a=2), axis=AX.X, op=ALU.add), [lsc, sc], [sc])
    op("act", lambda: nc.scalar.activation(out=sc.ap[:, 10:12], in_=sc.ap[:, 8:10], func=AF.Exp), [sc], [sc])
    op("dve", lambda: nc.vector.tensor_tensor(out=sc.ap[:, 0:1], in0=sc.ap[:, 10:11], in1=sc.ap[:, 11:12], op=ALU.subtract), [sc], [sc])
    op("dve", lambda: nc.vector.tensor_scalar(out=sc.ap[:, 0:1], in0=sc.ap[:, 0:1], scalar1=LAM_INIT, scalar2=None, op0=ALU.add), [sc], [sc])
    op("dve", lambda: nc.vector.tensor_scalar(out=sc.ap[:, 1:2], in0=sc.ap[:, 0:1], scalar1=-1.0, scalar2=None, op0=ALU.mult), [sc], [sc])
    op("dve", lambda: nc.vector.tensor_scalar(out=sc.ap[:, 5:6], in0=cst.ap[:, 649:650], scalar1=-1.0, scalar2=None, op0=ALU.mult), [sc, cst], [sc])
    op("dve", lambda: nc.vector.tensor_copy(out=sc.ap[:, 6:7], in_=cst.ap[:, 648:649]), [sc, cst], [sc])

    import os
    KSTOP = os.environ.get("KSTOP", "")
    RET = lambda: (C.barrier(), (nc, C, {}))[1]
    if KSTOP == "const":
        return RET()
    PS = [C.ps(f"ps{i}", [128, 512], F32) for i in range(8)]

    def psbf(i):
        return PS[i].ap[:].bitcast(BF16)

    def rmsnorm_rows(xb, hout, gidx, tmp, stat, nb_cols=D, gb=None):
        op("act", lambda: nc.scalar.activation(out=tmp.ap[:], in_=xb.ap[:], func=AF.Square, accum_out=stat.ap[:, 0:1]), [xb], [tmp, stat])
        op("act", lambda: nc.scalar.activation(out=stat.ap[:, 1:2], in_=stat.ap[:, 0:1], func=AF.Ln, scale=1.0 / nb_cols, bias=sc.ap[:, 2:3]), [stat, sc], [stat])
        op("act", lambda: nc.scalar.activation(out=stat.ap[:, 2:3], in_=stat.ap[:, 1:2], func=AF.Exp, scale=-0.5), [stat], [stat])
        op("dve", lambda: nc.vector.scalar_tensor_tensor(out=hout.ap[:], in0=xb.ap[:], scalar=stat.ap[:, 2:3], in1=gb.ap[:, gidx, :], op0=ALU.mult, op1=ALU.mult), [xb, stat, gb], [hout])

    mkA1 = C.mark()
    gb = C.sb("gbA", [128, 1, D], F32)
    dma("sp", gb.ap[:], gb_d.ap[:, 0:1, :], reads=[gb_d], writes=[gb])
    wA = C.sb("wA", [128, 16, NA], BF16)
    dma("sp", wA.ap[:], wA_bf.ap.rearrange("(c p) n -> p c n", p=128), reads=[wA_bf], writes=[wA])
    wrep = C.sb("wrep", [128, 16, 2, 128], BF16)
    for j in range(2):
        op("pool", lambda j=j: nc.gpsimd.tensor_copy(out=wrep.ap[:, :, j, :], in_=wA.ap[:, :, 2560 + j:2561 + j].to_broadcast([128, 16, 128])), [wA], [wrep])

    xbuf = [C.sb(f"xb{i}", [128, D], F32) for i in range(2)]
    sqt = C.sb("sqt", [128, D], BF16)
    hbuf = [C.sb(f"hb{i}", [128, D], BF16) for i in range(2)]
    hT = [C.sb(f"hT{i}", [128, 16, 512], BF16) for i in range(1)]
    stats = [C.sb(f"st{i}", [128, 4], F32) for i in range(4)]
    fo = [C.sb(f"fo{i}", [128, 512], BF16) for i in range(4)]
    to = [C.sb(f"to{i}", [128, 512], BF16) for i in range(2)]
    tov = [C.sb(f"tov{i}", [128, 256], BF16) for i in range(2)]
    tof = [C.sb(f"tof{i}", [128, 256], F32) for i in range(2)]
    gto = [C.sb(f"gto{i}", [128, 512], F32) for i in range(2)]
    cpre = [C.sb(f"cpre{i}", [128, 3 + 512], F32) for i in range(4)]
    cacc = [C.sb(f"cacc{i}", [128, 512], F32) for i in range(2)]
    for i in range(4):
        op("dve", lambda i=i: nc.vector.memset(cpre[i].ap[:], 0.0), [], [cpre[i]])

    xi = 0
    for t in range(NT):
        hTt = hT[0]
        for blk in range(4):
            r0 = t * 512 + blk * 128
            xb = xbuf[xi % 2]
            hbf = hbuf[xi % 2]
            st = stats[xi % 4]
            xi += 1
            dma("sp", xb.ap[:], x_d.ap[r0:r0 + 128, :], reads=[x_d], writes=[xb])
            rmsnorm_rows(xb, hbf, 0, sqt, st, gb=gb)
            for half in range(2):
                pb = PS[half]
                for c8 in range(8):
                    c = half * 8 + c8
                    op("pe", lambda c=c, c8=c8, half=half: nc.tensor.transpose(out=psbf(half)[:, c8 * 128:(c8 + 1) * 128], in_=hbf.ap[:, c * 128:(c + 1) * 128], identity=identb.ap[:]), [hbf, identb], [pb])
                src = psbf(half).rearrange("p (c t) -> p c t", c=8)
                dst = hTt.ap[:, half * 8:(half + 1) * 8, blk * 128:(blk + 1) * 128]
                if half == 0:
                    op("act", lambda src=src, dst=dst: nc.scalar.copy(out=dst, in_=src), [pb], [hTt])
                else:
                    op("dve", lambda src=src, dst=dst: nc.vector.tensor_copy(out=dst, in_=src), [pb], [hTt])
        if KSTOP == "xT":
            return RET()
        t0 = t * 512
        pi = 2
        for oc in range(8):
            pb = PS[2 + (oc % 2)]
            for kc in range(16):
                op("pe", lambda oc=oc, kc=kc, pb=pb: nc.tensor.matmul(pb.ap[:], lhsT=wA.ap[:, kc, oc * 128:(oc + 1) * 128], rhs=hTt.ap[:, kc, :], start=(kc == 0), stop=(kc == 15)), [wA, hTt], [pb])
            f = fo[oc % 4]
            if oc % 2 == 0:
                op("act", lambda f=f, pb=pb: nc.scalar.copy(out=f.ap[:], in_=pb.ap[:]), [pb], [f])
            else:
                op("dve", lambda f=f, pb=pb: nc.vector.tensor_copy(out=f.ap[:], in_=pb.ap[:]), [pb], [f])
            dst = (qT_s if oc < 4 else kT_s)
            dma("sp", dst.ap[oc % 4, :, t0:t0 + 512], f.ap[:], reads=[f], writes=[dst], multi=True, owner=f)
        if KSTOP == "qk":
            return RET()
        for j in range(4):
            pb = PS[4 + (j % 2)]
            col0 = 1536 + j * 128
            for kc in range(16):
                op("pe", lambda kc=kc, pb=pb, col0=col0: nc.tensor.matmul(pb.ap[:], lhsT=wA.ap[:, kc, col0:col0 + 128], rhs=hTt.ap[:, kc, :], start=(kc == 0), stop=(kc == 15)), [wA, hTt], [pb])
            cp = cpre[j]
            ca = cacc[j % 2]
            op("act", lambda cp=cp, pb=pb: nc.scalar.copy(out=cp.ap[:, 3:515], in_=pb.ap[:]), [pb], [cp])
            wcol = 48 + j * 4
            op("dve", lambda cp=cp, ca=ca, wcol=wcol, j=j: nc.vector.tensor_scalar(out=ca.ap[:], in0=cp.ap[:, 0:512], scalar1=fm.ap[:, wcol:wcol + 1], scalar2=cst.ap[:, 644 + j:645 + j], op0=ALU.mult, op1=ALU.add), [cp, fm, cst], [ca])
            for tap in range(1, 4):
                op("dve", lambda cp=cp, ca=ca, wcol=wcol, tap=tap: nc.vector.scalar_tensor_tensor(out=ca.ap[:], in0=cp.ap[:, tap:tap + 512], scalar=fm.ap[:, wcol + tap:wcol + tap + 1], in1=ca.ap[:], op0=ALU.mult, op1=ALU.add), [cp, fm, ca], [ca])
            op("pool", lambda cp=cp: nc.gpsimd.tensor_copy(out=cp.ap[:, 0:3], in_=cp.ap[:, 512:515]), [cp], [cp])
            f = fo[j]
            op("act", lambda f=f, ca=ca, j=j: nc.scalar.activation(out=f.ap[:], in_=ca.ap[:], func=AF.Silu), [ca], [f])
            if j >= 2:
                op("pool", lambda f=f: nc.gpsimd.tensor_scalar(out=f.ap[:], in0=f.ap[:], scalar1=1.0 / 16.0, scalar2=None, op0=ALU.mult), [f], [f])
            dst = (mq_s if j < 2 else mk_s)
            dma("sp", dst.ap[j % 2, :, t0:t0 + 512], f.ap[:], reads=[f], writes=[dst], multi=True, owner=f)
        if KSTOP == "conv":
            return RET()
        for j in range(2):
            pb = PS[6 + j]
            for kc in range(16):
                op("pe", lambda kc=kc, pb=pb, j=j: nc.tensor.matmul(pb.ap[:], lhsT=wrep.ap[:, kc, j, :], rhs=hTt.ap[:, kc, :], start=(kc == 0), stop=(kc == 15)), [wrep, hTt], [pb])
            g = gto[j]
            op("act", lambda g=g, pb=pb: nc.scalar.copy(out=g.ap[:], in_=pb.ap[:]), [pb], [g])
            dma("sp", gt_s.ap[j, :, t0:t0 + 512], g.ap[:], reads=[g], writes=[gt_s], multi=True, owner=g)
        if KSTOP == "gates":
            return RET()
        for blk in range(4):
            r0 = t0 + blk * 128
            pb = PS[2 + (blk % 2)]
            for kc in range(16):
                op("pe", lambda kc=kc, pb=pb, blk=blk: nc.tensor.matmul(pb.ap[:], lhsT=hTt.ap[:, kc, blk * 128:(blk + 1) * 128], rhs=wA.ap[:, kc, 1024:1536], start=(kc == 0), stop=(kc == 15)), [wA, hTt], [pb])
            tb = to[blk % 2]
            op("dve", lambda tb=tb, pb=pb: nc.vector.tensor_copy(out=tb.ap[:], in_=pb.ap[:]), [pb], [tb])
            for hh in range(2):
                dma("sp", v_s.ap[hh, r0:r0 + 128, :], tb.ap[:, hh * 256:(hh + 1) * 256], reads=[tb], writes=[v_s], multi=True, owner=tb)
            pb2 = PS[4 + (blk % 2)]
            for kc in range(16):
                op("pe", lambda kc=kc, pb2=pb2, blk=blk: nc.tensor.matmul(pb2.ap[:], lhsT=hTt.ap[:, kc, blk * 128:(blk + 1) * 128], rhs=wA.ap[:, kc, 2048:2560], start=(kc == 0), stop=(kc == 15)), [wA, hTt], [pb2])
            tv = tov[blk % 2]
            tf = tof[blk % 2]
            op("dve", lambda tv=tv, pb2=pb2: nc.vector.tensor_copy(out=tv.ap[:], in_=pb2.ap[:, 0:256]), [pb2], [tv])
            op("act", lambda tf=tf, pb2=pb2: nc.scalar.activation(out=tf.ap[:], in_=pb2.ap[:, 256:512], func=AF.Sigmoid), [pb2], [tf])
            dma("sp", mv_s.ap[r0:r0 + 128, :], tv.ap[:], reads=[tv], writes=[mv_s], multi=True, owner=tv)
            dma("sp", mo_s.ap[r0:r0 + 128, :], tf.ap[:], reads=[tf], writes=[mo_s], multi=True, owner=tf)
        if KSTOP == "tile0":
            return RET()

    if full:
        other_casts()
    C.release(mkA1)
    mk2 = C.mark()
    KT = C.sb("KT", [128, 2, S], BF16)
    VV = C.sb("VV", [128, NBLK, 257], BF16)
    op("dve", lambda: nc.vector.memset(VV.ap[:, :, 256:257], 1.0), [], [VV])
    QT = [C.sb(f"QT{i}", [128, 2, 256], BF16) for i in range(3)]
    Pb = [C.sb(f"Pb{i}", [128, 2, 256], BF16) for i in range(3)]
    o1 = [C.sb(f"o1_{i}", [128, 256], F32) for i in range(2)]
    osq = C.sb("osq", [128, 256], BF16)
    dab = [C.sb(f"dab{i}", [128, 256], BF16) for i in range(2)]
    daT = [C.sb(f"daT{i}", [128, 2, 128], BF16) for i in range(2)]
    ast = [C.sb(f"ast{i}", [128, 8], F32) for i in range(4)]
    NQT = S // 256
    SCL = 128 ** -0.5
    tri3 = trib.ap[:].unsqueeze(1).to_broadcast([128, 2, 128])
    si = 0
    fi = 0
    qi = 0
    for hl in range(2):
        rep_h = 3 if hl == 0 else 7
        dma("sp", KT.ap[:, 0, :], kT_s.ap[hl * 2], reads=[kT_s], writes=[KT])
        dma("sp", KT.ap[:, 1, :], kT_s.ap[hl * 2 + 1], reads=[kT_s], writes=[KT], multi=True)
        for n0 in range(0, NBLK, 16):
            dma("sp", VV.ap[:, n0:n0 + 16, 0:256], v_s.ap[hl, n0 * 128:(n0 + 16) * 128, :].rearrange("(n p) d -> p n d", p=128), reads=[v_s], writes=[VV], multi=True)
        for t in range(NQT):
            kbs = kb_list(rep_h, t, NBLK)
            qt = QT[qi % 3]
            qi += 1
            dma("sp", qt.ap[:], qT_s.ap[hl * 2:hl * 2 + 2, :, t * 256:(t + 1) * 256].rearrange("m d q -> d m q"), reads=[qT_s], writes=[qt])
            acc = [[PS[2], PS[3]], [PS[4], PS[5]]]
            for kb in kbs:
                d = kb - 2 * t
                qlo = 128 if d == 1 else 0
                sbk = PS[si % 2]
                P = Pb[si % 3]
                si += 1
                for m in range(2):
                    op("pe", lambda m=m, sbk=sbk, kb=kb, qt=qt, qlo=qlo: nc.tensor.matmul(sbk.ap[:, m * 256 + qlo:(m + 1) * 256], lhsT=KT.ap[:, m, kb * 128:(kb + 1) * 128], rhs=qt.ap[:, m, qlo:256], start=True, stop=True), [KT, qt], [sbk])
                S3 = sbk.ap[:].rearrange("p (m q) -> p m q", m=2)
                op("act", lambda P=P, S3=S3, qlo=qlo, d=d, hl=hl: nc.scalar.activation(out=P.ap[:, :, qlo:256], in_=S3[:, :, qlo:256], func=AF.Exp, bias=alibi.ap[:, hl, d + 128:d + 129], scale=SCL), [sbk, alibi], [P])
                if d >= 0:
                    dq = 0 if d == 0 else 128
                    op("dve", lambda P=P, dq=dq: nc.vector.tensor_tensor(out=P.ap[:, :, dq:dq + 128], in0=P.ap[:, :, dq:dq + 128], in1=tri3, op=ALU.mult), [P, trib], [P])
                for m in range(2):
                    for qb in range(2):
                        if d == 1 and qb == 0:
                            continue
                        a = acc[m][qb]
                        is_first = (kb == kbs[0])
                        is_last = (d == 0) if qb == 0 else (kb == kbs[-1])
                        op("pe", lambda a=a, P=P, m=m, qb=qb, kb=kb, is_first=is_first, is_last=is_last: nc.tensor.matmul(a.ap[:, 0:257], lhsT=P.ap[:, m, qb * 128:(qb + 1) * 128], rhs=VV.ap[:, kb, :], start=is_first, stop=is_last), [P, VV], [a])
            for qb in range(2):
                a1, a2 = acc[0][qb], acc[1][qb]
                st_ = ast[fi % 4]
                o = o1[fi % 2]
                db = dab[fi % 2]
                dT = daT[fi % 2]
                pbT = PS[6 + fi % 2]
                fi += 1
                op("dve", lambda st_=st_, a1=a1: nc.vector.reciprocal(out=st_.ap[:, 0:1], in_=a1.ap[:, 256:257]), [a1], [st_])
                op("dve", lambda st_=st_, a2=a2: nc.vector.reciprocal(out=st_.ap[:, 1:2], in_=a2.ap[:, 256:257]), [a2, st_], [st_])
                op("dve", lambda st_=st_: nc.vector.tensor_tensor(out=st_.ap[:, 2:3], in0=st_.ap[:, 1:2], in1=sc.ap[:, 1:2], op=ALU.mult), [st_, sc], [st_])
                op("act", lambda o=o, a1=a1, st_=st_: nc.scalar.activation(out=o.ap[:], in_=a1.ap[:, 0:256], func=AF.Copy, scale=st_.ap[:, 0:1]), [a1, st_], [o])
                op("dve", lambda o=o, a2=a2, st_=st_: nc.vector.scalar_tensor_tensor(out=o.ap[:], in0=a2.ap[:, 0:256], scalar=st_.ap[:, 2:3], in1=o.ap[:], op0=ALU.mult, op1=ALU.add), [a2, st_, o], [o])
                op("act", lambda o=o, st_=st_: nc.scalar.activation(out=osq.ap[:], in_=o.ap[:], func=AF.Square, accum_out=st_.ap[:, 3:4]), [o, st_], [osq, st_])
                op("act", lambda st_=st_: nc.scalar.activation(out=st_.ap[:, 4:5], in_=st_.ap[:, 3:4], func=AF.Ln, scale=1.0 / 256, bias=sc.ap[:, 2:3]), [st_, sc], [st_])
                op("act", lambda st_=st_: nc.scalar.activation(out=st_.ap[:, 5:6], in_=st_.ap[:, 4:5], func=AF.Exp, scale=-0.5), [st_], [st_])
                op("dve", lambda st_=st_: nc.vector.tensor_scalar(out=st_.ap[:, 5:6], in0=st_.ap[:, 5:6], scalar1=(1.0 - LAM_INIT), scalar2=None, op0=ALU.mult), [st_], [st_])
                op("dve", lambda db=db, o=o, st_=st_, hl=hl: nc.vector.scalar_tensor_tensor(out=db.ap[:], in0=o.ap[:], scalar=st_.ap[:, 5:6], in1=hb.ap[:, hl * 256:(hl + 1) * 256], op0=ALU.mult, op1=ALU.mult), [o, st_, hb], [db])
                for c in range(2):
                    op("pe", lambda c=c, db=db, pbT=pbT: nc.tensor.transpose(out=pbT.ap[:].bitcast(BF16)[:, c * 128:(c + 1) * 128], in_=db.ap[:, c * 128:(c + 1) * 128], identity=identb.ap[:]), [db, identb], [pbT])
                op("act", lambda dT=dT, pbT=pbT: nc.scalar.copy(out=dT.ap[:].rearrange("p c t -> p (c t)"), in_=pbT.ap[:].bitcast(BF16)[:, 0:256]), [pbT], [dT])
                tok0 = t * 256 + qb * 128
                dma("sp", exi.ap[t, hl * 256:(hl + 1) * 256, qb * 128:(qb + 1) * 128].rearrange("(c p) t -> p c t", p=128), dT.ap[:], reads=[dT], writes=[exi], multi=True, owner=dT)
    if KSTOP == "A2":
        return RET()
    C.release(mk2)
    mk3 = C.mark()
    SEGL = min(S, 2048)
    NSEG = S // SEGL
    NCH = SEGL // 128
    onesr = C.sb("onesr", [128, SEGL], F32)
    op("dve", lambda: nc.vector.memset(onesr.ap[:], 1.0), [], [onesr])
    igb = C.sb("igb", [128, SEGL], F32)
    fgb = C.sb("fgb", [128, SEGL], F32)
    Gb = C.sb("Gb", [128, SEGL], F32)
    ub = C.sb("ub", [128, SEGL], F32)
    mb_ = C.sb("mb_", [128, SEGL], F32)
    tmp3 = C.sb("tmp3", [128, SEGL], F32)
    Pext = C.sb("Pext", [128, SEGL + 1], F32)
    carry = C.sb("carry", [128, 2], F32)
    op("dve", lambda: nc.vector.memset(carry.ap[:], 0.0), [], [carry])
    ucol = C.sb("ucol", [128, NCH], F32)
    mcol = C.sb("mcol", [128, NCH], F32)
    emc = C.sb("emc", [128, NCH], F32)
    wacol = C.sb("wacol", [128, NCH], F32)
    dcol = C.sb("dcol", [128, NCH], F32)
    t16 = C.sb("t16", [128, NCH], F32)
    t16b = C.sb("t16b", [128, NCH], F32)
    Cst = C.sb("Cst", [128, 2, 257], F32)
    Cbf = C.sb("Cbf", [128, 2, 257], BF16)
    op("dve", lambda: nc.vector.memset(Cst.ap[:], 0.0), [], [Cst])
    op("dve", lambda: nc.vector.memset(Cbf.ap[:], 0.0), [], [Cbf])
    R2 = 2
    mqb = [C.sb(f"mqb{i}", [128, 2, 128], BF16) for i in range(R2)]
    mkb = [C.sb(f"mkb{i}", [128, 2, 128], BF16) for i in range(R2)]
    mvb = [C.sb(f"mvb{i}", [128, 257], BF16) for i in range(R2)]
    for i in range(R2):
        op("dve", lambda i=i: nc.vector.memset(mvb[i].ap[:, 256:257], 1.0), [], [mvb[i]])
    mob = [C.sb(f"mob{i}", [128, 256], F32) for i in range(R2)]
    irb = [C.sb(f"irb{i}", [128, 128], F32) for i in range(R2)]
    qpb = [C.sb(f"qpb{i}", [128, 2, 128], BF16) for i in range(R2)]
    Ttb = [C.sb(f"Ttb{i}", [128, 128], F32) for i in range(R2)]
    Dtb = [C.sb(f"Dtb{i}", [128, 128], F32) for i in range(R2)]
    Wtb = [C.sb(f"Wtb{i}", [128, 128], BF16) for i in range(R2)]
    ktmb = [C.sb(f"ktmb{i}", [128, 256], BF16) for i in range(R2)]
    vpb = [C.sb(f"vpb{i}", [128, 257], BF16) for i in range(R2)]
    fsb = [C.sb(f"fsb{i}", [128, 8], F32) for i in range(R2)]
    hhb = [C.sb(f"hhb{i}", [128, 256], F32) for i in range(R2)]
    hsq = C.sb("hsq", [128, 256], BF16)
    hh2b = [C.sb(f"hh2b{i}", [128, 256], F32) for i in range(R2)]
    hmbb = [C.sb(f"hmbb{i}", [128, 256], BF16) for i in range(R2)]
    hmTb = [C.sb(f"hmTb{i}", [128, 2, 128], BF16) for i in range(R2)]
    id3 = identf.ap[:].unsqueeze(1).to_broadcast([128, NCH, 128])
    ci = 0
    for sg in range(NSEG):
        s0 = sg * SEGL
        dma("sp", igb.ap[:], gt_s.ap[0, :, s0:s0 + SEGL], reads=[gt_s], writes=[igb])
        dma("sp", fgb.ap[:], gt_s.ap[1, :, s0:s0 + SEGL], reads=[gt_s], writes=[fgb])
        op("act", lambda: nc.scalar.activation(out=fgb.ap[:], in_=fgb.ap[:], func=AF.Exp, scale=-1.0, bias=sc.ap[:, 5:6]), [fgb, sc], [fgb])
        op("act", lambda: nc.scalar.activation(out=fgb.ap[:], in_=fgb.ap[:], func=AF.Ln, bias=sc.ap[:, 3:4]), [fgb, sc], [fgb])
        op("dve", lambda: nc.vector.tensor_tensor_scan(out=Gb.ap[:], data0=onesr.ap[:], data1=fgb.ap[:], initial=carry.ap[:, 0:1], op0=ALU.mult, op1=ALU.add), [onesr, fgb, carry], [Gb])
        op("dve", lambda: nc.vector.scalar_tensor_tensor(out=ub.ap[:], in0=igb.ap[:], scalar=sc.ap[:, 6:7], in1=Gb.ap[:], op0=ALU.add, op1=ALU.add), [igb, sc, Gb], [ub])
        op("dve", lambda: nc.vector.tensor_copy(out=Pext.ap[:, 0:1], in_=carry.ap[:, 1:2]), [carry], [Pext])
        op("dve", lambda: nc.vector.tensor_tensor_scan(out=Pext.ap[:, 1:SEGL + 1], data0=onesr.ap[:], data1=ub.ap[:], initial=carry.ap[:, 1:2], op0=ALU.mult, op1=ALU.max), [onesr, ub, carry, Pext], [Pext])
        op("dve", lambda: nc.vector.tensor_copy(out=carry.ap[:, 0:1], in_=Gb.ap[:, SEGL - 1:SEGL]), [Gb, carry], [carry])
        op("dve", lambda: nc.vector.tensor_copy(out=carry.ap[:, 1:2], in_=Pext.ap[:, SEGL:SEGL + 1]), [Pext, carry], [carry])
        op("dve", lambda: nc.vector.tensor_tensor(out=mb_.ap[:], in0=Pext.ap[:, 1:SEGL + 1], in1=Gb.ap[:], op=ALU.subtract), [Pext, Gb], [mb_])
        for (srcb, dstc) in ((ub, ucol), (mb_, mcol)):
            op("dve", lambda srcb=srcb: nc.vector.tensor_tensor(out=tmp3.ap[:].rearrange("p (c t) -> p c t", t=128), in0=srcb.ap[:].rearrange("p (c t) -> p c t", t=128), in1=id3, op=ALU.mult), [srcb, identf], [tmp3])
            op("dve", lambda dstc=dstc: nc.vector.tensor_reduce(out=dstc.ap[:], in_=tmp3.ap[:].rearrange("p (c t) -> p c t", t=128), axis=AX.X, op=ALU.add), [tmp3], [dstc])
        op("act", lambda: nc.scalar.activation(out=emc.ap[:], in_=mcol.ap[:], func=AF.Exp, scale=-1.0), [mcol], [emc])
        op("dve", lambda: nc.vector.tensor_tensor(out=t16.ap[:], in0=ucol.ap[:], in1=Pext.ap[:, 128:SEGL + 1:128], op=ALU.subtract), [ucol, Pext], [t16])
        op("act", lambda: nc.scalar.activation(out=wacol.ap[:], in_=t16.ap[:], func=AF.Exp), [t16], [wacol])
        op("dve", lambda: nc.vector.tensor_tensor(out=t16b.ap[:], in0=Pext.ap[:, 0:SEGL:128], in1=Pext.ap[:, 128:SEGL + 1:128], op=ALU.subtract), [Pext], [t16b])
        op("act", lambda: nc.scalar.activation(out=dcol.ap[:], in_=t16b.ap[:], func=AF.Exp), [t16b], [dcol])
        for j in range(NCH):
            tk0 = s0 + j * 128
            r = ci % R2
            ci += 1
            mq, mk, mv, mo, ir, qp, Tt, Dt, Wt, ktm, vp, fs, hh, hh2, hmb, hmT = (mqb[r], mkb[r], mvb[r], mob[r], irb[r], qpb[r], Ttb[r], Dtb[r], Wtb[r], ktmb[r], vpb[r], fsb[r], hhb[r], hh2b[r], hmbb[r], hmTb[r])
            dma("sp", mq.ap[:], mq_s.ap[:, :, tk0:tk0 + 128].rearrange("c d t -> d c t"), reads=[mq_s], writes=[mq])
            dma("sp", mk.ap[:], mk_s.ap[:, :, tk0:tk0 + 128].rearrange("c d t -> d c t"), reads=[mk_s], writes=[mk])
            dma("sp", mv.ap[:, 0:256], mv_s.ap[tk0:tk0 + 128, :], reads=[mv_s], writes=[mv])
            dma("sp", mo.ap[:], mo_s.ap[tk0:tk0 + 128, :], reads=[mo_s], writes=[mo])
            Pch = Pext.ap[:, 1 + j * 128:1 + (j + 1) * 128]
            op("act", lambda ir=ir, Pch=Pch, j=j: nc.scalar.activation(out=ir.ap[:], in_=Pch, func=AF.Exp, scale=-1.0, bias=Pext.ap[:, j * 128:j * 128 + 1]), [Pext], [ir])
            op("dve", lambda qp=qp, mq=mq, ir=ir: nc.vector.tensor_tensor(out=qp.ap[:], in0=mq.ap[:], in1=ir.ap[:].unsqueeze(1).to_broadcast([128, 2, 128]), op=ALU.mult), [mq, ir], [qp])
            op("dve", lambda Tt=Tt, Pch=Pch: nc.vector.tensor_tensor(out=Tt.ap[:], in0=maskneg.ap[:], in1=Pch, op=ALU.subtract), [maskneg, Pext], [Tt])
            op("act", lambda Dt=Dt, Tt=Tt, j=j: nc.scalar.activation(out=Dt.ap[:], in_=Tt.ap[:], func=AF.Exp, bias=ucol.ap[:, j:j + 1]), [Tt, ucol], [Dt])
            for c in range(2):
                op("pe", lambda c=c, mk=mk, mq=mq: nc.tensor.matmul(PS[0].ap[:, 0:128], lhsT=mk.ap[:, c, :], rhs=mq.ap[:, c, :], start=(c == 0), stop=(c == 1)), [mk, mq], [PS[0]])
            op("dve", lambda Wt=Wt, Dt=Dt: nc.vector.tensor_tensor(out=Wt.ap[:], in0=PS[0].ap[:, 0:128], in1=Dt.ap[:], op=ALU.mult), [PS[0], Dt], [Wt])
            op("pe", lambda Wt=Wt, mv=mv: nc.tensor.matmul(PS[1].ap[:, 0:257], lhsT=Wt.ap[:], rhs=mv.ap[:, 0:257], start=True, stop=False), [Wt, mv], [PS[1]])
            for c in range(2):
                op("pe", lambda c=c, qp=qp: nc.tensor.matmul(PS[1].ap[:, 0:257], lhsT=qp.ap[:, c, :], rhs=Cbf.ap[:, c, :], start=False, stop=(c == 1)), [qp, Cbf], [PS[1]])
            for c in range(2):
                op("pe", lambda c=c, mk=mk: nc.tensor.transpose(out=psbf(2)[:, c * 128:(c + 1) * 128], in_=mk.ap[:, c, :], identity=identb.ap[:]), [mk, identb], [PS[2]])
            op("act", lambda ktm=ktm: nc.scalar.copy(out=ktm.ap[:], in_=psbf(2)[:, 0:256]), [PS[2]], [ktm])
            op("dve", lambda vp=vp, mv=mv, j=j: nc.vector.tensor_scalar(out=vp.ap[:], in0=mv.ap[:], scalar1=wacol.ap[:, j:j + 1], scalar2=None, op0=ALU.mult), [mv, wacol], [vp])
            for c in range(2):
                op("pe", lambda c=c, ktm=ktm, vp=vp: nc.tensor.matmul(PS[3 + c].ap[:, 0:257], lhsT=ktm.ap[:, c * 128:(c + 1) * 128], rhs=vp.ap[:], start=True, stop=True), [ktm, vp], [PS[3 + c]])
            for c in range(2):
                op("dve", lambda c=c, j=j: nc.vector.scalar_tensor_tensor(out=Cst.ap[:, c, :], in0=Cst.ap[:, c, :], scalar=dcol.ap[:, j:j + 1], in1=PS[3 + c].ap[:, 0:257], op0=ALU.mult, op1=ALU.add), [dcol, PS[3 + c]], [Cst])
            op("act", lambda: nc.scalar.copy(out=Cbf.ap[:], in_=Cst.ap[:]), [Cst], [Cbf])
            op("act", lambda fs=fs: nc.scalar.activation(out=fs.ap[:, 0:1], in_=PS[1].ap[:, 256:257], func=AF.Abs), [PS[1]], [fs])
            op("dve", lambda fs=fs, j=j: nc.vector.tensor_tensor(out=fs.ap[:, 0:1], in0=fs.ap[:, 0:1], in1=emc.ap[:, j:j + 1], op=ALU.max), [fs, emc], [fs])
            op("dve", lambda fs=fs: nc.vector.reciprocal(out=fs.ap[:, 1:2], in_=fs.ap[:, 0:1]), [fs], [fs])
            op("act", lambda hh=hh, fs=fs: nc.scalar.activation(out=hh.ap[:], in_=PS[1].ap[:, 0:256], func=AF.Copy, scale=fs.ap[:, 1:2]), [PS[1], fs], [hh])
            op("act", lambda hh=hh, fs=fs: nc.scalar.activation(out=hsq.ap[:], in_=hh.ap[:], func=AF.Square, accum_out=fs.ap[:, 2:3]), [hh, fs], [hsq, fs])
            op("act", lambda fs=fs: nc.scalar.activation(out=fs.ap[:, 3:4], in_=fs.ap[:, 2:3], func=AF.Ln, scale=1.0 / 256, bias=sc.ap[:, 2:3]), [fs, sc], [fs])
            op("act", lambda fs=fs: nc.scalar.activation(out=fs.ap[:, 4:5], in_=fs.ap[:, 3:4], func=AF.Exp, scale=-0.5), [fs], [fs])
            op("dve", lambda hh2=hh2, hh=hh, fs=fs: nc.vector.scalar_tensor_tensor(out=hh2.ap[:], in0=hh.ap[:], scalar=fs.ap[:, 4:5], in1=hb.ap[:, 512:768], op0=ALU.mult, op1=ALU.mult), [hh, fs, hb], [hh2])
            op("dve", lambda hmb=hmb, hh2=hh2, mo=mo: nc.vector.tensor_tensor(out=hmb.ap[:], in0=hh2.ap[:], in1=mo.ap[:], op=ALU.mult), [hh2, mo], [hmb])
            for c in range(2):
                op("pe", lambda c=c, hmb=hmb: nc.tensor.transpose(out=psbf(5)[:, c * 128:(c + 1) * 128], in_=hmb.ap[:, c * 128:(c + 1) * 128], identity=identb.ap[:]), [hmb, identb], [PS[5]])
            op("act", lambda hmT=hmT: nc.scalar.copy(out=hmT.ap[:].rearrange("p c t -> p (c t)"), in_=psbf(5)[:, 0:256]), [PS[5]], [hmT])
            dma("sp", exi.ap[tk0 // 256, 512:768, (tk0 % 256):(tk0 % 256) + 128].rearrange("(c p) t -> p c t", p=128), hmT.ap[:], reads=[hmT], writes=[exi], multi=True, owner=hmT)
    if KSTOP == "A3":
        return RET()
    C.release(mk3)
    if dbg and "exi_dbg" in dbg:
        exi_dbg = C.dram("exi_dbg", [768, S], BF16, kind="ExternalOutput")
        for ch in range(NXC):
            for r0 in range(0, 768, 128):
                dma("sp", exi_dbg.ap[r0:r0 + 128, ch * 256:(ch + 1) * 256], exi.ap[ch, r0:r0 + 128, :], reads=[exi], writes=[exi_dbg], multi=True)
    if not full:
        return RET()
    xs_ = nc.semaphore("xsem").__enter__()
    for ch in range(NXC):
        nc.gpsimd.collective_compute("AllGather", ALU.bypass, replica_groups=[[0, 1, 2, 3], [4, 5, 6, 7]], ins=[exi.ap[ch]], outs=[exo.ap[ch]]).then_inc(xs_)
    for e in C.eng:
        C.eng[e].wait_ge(xs_, NXC)
    if KSTOP == "X":
        return RET()
    for cs in ccs:
        for e in C.eng:
            C.eng[e].wait_ge(cs, 1)
    TB = 512
    NTB = SEG // TB
    if dbg and "x1_dbg" in dbg:
        x1_dbg = C.dram("x1_dbg", [SEG, D], F32, kind="ExternalOutput")
        rw_dbg = C.dram("rw_dbg", [SEG, 32], F32, kind="ExternalOutput")
    xt = C.sb("xt", [128, 4, D], F32)
    h2T = C.sb("h2T", [128, 16, TB], BF16)
    mT = C.sb("mT", [128, 16, 256], BF16)
    mkT = C.sb("mkT", [128, 8, 256], BF16)
    mvv = C.sb("mvv", [128, 2, 4, 257], BF16)
    wr_sb = C.sb("wr_sb", [128, 16, 36], F32)
    rw = C.sb("rw", [128, 4, 32], F32)
    onesb = C.sb("onesb", [128, 128], BF16)
    bst = [C.sb(f"bst{i}", [128, 8], F32) for i in range(4)]
    op("pool", lambda: nc.gpsimd.memset(onesb.ap[:], 1.0), [], [onesb])
    op("pool", lambda: nc.gpsimd.memset(mvv.ap[:], 1.0), [], [mvv])
    dma("sp", wr_sb.ap[:], wr_d.ap.rearrange("(c p) n -> p c n", p=128), reads=[wr_d], writes=[wr_sb])
    bsi = [0]

    def nst():
        bsi[0] += 1
        return bst[bsi[0] % 4]

    def transposes_bf(hbf, dst3, col0, ident=identb):
        for half in range(2):
            pb = PS[half]
            for c8 in range(8):
                c = half * 8 + c8
                op("pe", lambda c=c, c8=c8, half=half: nc.tensor.transpose(out=psbf(half)[:, c8 * 128:(c8 + 1) * 128], in_=hbf.ap[:, c * 128:(c + 1) * 128], identity=ident.ap[:]), [hbf, ident], [pb])
            src = psbf(half).rearrange("p (c t) -> p c t", c=8)
            dst = dst3.ap[:, half * 8:(half + 1) * 8, col0:col0 + 128]
            if half == 0:
                op("act", lambda src=src, dst=dst: nc.scalar.copy(out=dst, in_=src), [pb], [dst3])
            else:
                op("dve", lambda src=src, dst=dst: nc.vector.tensor_copy(out=dst, in_=src), [pb], [dst3])

    mk0 = C.mark()
    gmem = C.sb("gmem", [128, 1, D], F32)
    dma("sp", gmem.ap[:], gb_d.ap[:, 3:4, :], reads=[gb_d], writes=[gmem])
    memx = [C.sb(f"memx{i}", [128, D], F32) for i in range(2)]
    memh = [C.sb(f"memh{i}", [128, D], BF16) for i in range(2)]
    sqj = C.sb("sqj", [128, D], BF16)
    memT = C.sb("memT", [128, 16, 256], BF16)
    wbuf = [C.sb(f"wbuf{i}", [128, 8192], BF16) for i in range(2)]
    for blk in range(2):
        dma("sp", memx[blk].ap[:], mem_d.ap[blk * 128:(blk + 1) * 128, :], reads=[mem_d], writes=[memx[blk]])
        rmsnorm_rows(memx[blk], memh[blk], 0, sqj, nst(), gb=gmem)
        transposes_bf(memh[blk], memT, blk * 128)
    wi = 0
    for oc in range(8):
        wb = wbuf[wi % 2]
        wi += 1
        wb3 = wb.ap[:, 0:2048].rearrange("p (c n) -> p c n", n=128)
        dma("sp", wb3, v_wmk(oc), reads=[wmkt], writes=[wb])
        pb = PS[2 + oc % 2]
        for kc in range(16):
            op("pe", lambda kc=kc, pb=pb, wb3=wb3: nc.tensor.matmul(pb.ap[:, 0:256], lhsT=wb3[:, kc, :], rhs=memT.ap[:, kc, :], start=(kc == 0), stop=(kc == 15)), [wb, memT], [pb])
        op("act", lambda pb=pb, oc=oc: nc.scalar.copy(out=mkT.ap[:, oc, :], in_=pb.ap[:, 0:256]), [pb], [mkT])
    for ct in range(2):
        wb = wbuf[wi % 2]
        wi += 1
        wb3 = wb.ap[:].rearrange("p (c n) -> p c n", n=512)
        dma("sp", wb3, v_wmv(ct), reads=[wmvt], writes=[wb])
        for blk in range(2):
            pb = PS[4 + blk]
            for kc in range(16):
                op("pe", lambda kc=kc, pb=pb, wb3=wb3, blk=blk: nc.tensor.matmul(pb.ap[:], lhsT=memT.ap[:, kc, blk * 128:(blk + 1) * 128], rhs=wb3[:, kc, :], start=(kc == 0), stop=(kc == 15)), [wb, memT], [pb])
            op("dve", lambda pb=pb, blk=blk, ct=ct: nc.vector.tensor_copy(out=mvv.ap[:, blk, ct * 2:(ct + 1) * 2, 0:256], in_=pb.ap[:].rearrange("p (h d) -> p h d", h=2)), [pb], [mvv])
    C.release(mk0)
    if KSTOP == "B0":
        return RET()

    ohc = cst.ap[:, 652:656]
    for tb in range(NTB):
        tokb = tb * TB
        for blk in range(4):
            dma("sp", xt.ap[:, blk, :], xseg_d.ap[tokb + blk * 128:tokb + (blk + 1) * 128, :], reads=[xseg_d], writes=[xt], multi=(blk > 0))
        for hf in range(2):
            tok0 = tokb + hf * 256
            mk1 = C.mark()
            gmix = C.sb("gmix", [128, 1, D], F32)
            dma("sp", gmix.ap[:], gb_d.ap[:, 0:1, :], reads=[gb_d], writes=[gmix])
            hbb = [C.sb(f"hbb{i}", [128, D], BF16) for i in range(2)]
            sqj = C.sb("sqj1", [128, D], BF16)
            hT = C.sb("hTb", [128, 16, 256], BF16)
            exs = C.sb("exs", [128, 24, 256], BF16)
            cand = [C.sb(f"cand{i}", [128, 3, 256], BF16) for i in range(4)]
            xaq = C.sb("xaq", [128, 2, 256], BF16)
            Pm = C.sb("Pm", [128, 2, 256], BF16)
            xaT = C.sb("xaT", [128, 8, 256], BF16)
            rcp = C.sb("rcp", [128, 256], F32)
            wbuf = [C.sb(f"wbufa{i}", [128, 8192], BF16) for i in range(2)]
            sig = [C.sb(f"sig{i}", [128, 256], F32) for i in range(3)]
            tm = [C.sb(f"tm{i}", [128, 256], F32) for i in range(2)]
            for b2 in range(2):
                blk = hf * 2 + b2
                xv = Buf(xt.ap[:, blk, :], "xv")
                st = nst()
                op("act", lambda xv=xv, st=st: nc.scalar.activation(out=sqj.ap[:], in_=xv.ap, func=AF.Square, accum_out=st.ap[:, 0:1]), [xt, st], [sqj, st])
                op("act", lambda st=st: nc.scalar.activation(out=st.ap[:, 1:2], in_=st.ap[:, 0:1], func=AF.Ln, scale=1.0 / D, bias=sc.ap[:, 2:3]), [st, sc], [st])
                op("act", lambda st=st: nc.scalar.activation(out=st.ap[:, 2:3], in_=st.ap[:, 1:2], func=AF.Exp, scale=-0.5), [st], [st])
                op("dve", lambda xv=xv, st=st, b2=b2: nc.vector.scalar_tensor_tensor(out=hbb[b2].ap[:], in0=xv.ap, scalar=st.ap[:, 2:3], in1=gmix.ap[:, 0, :], op0=ALU.mult, op1=ALU.mult), [xt, st, gmix], [hbb[b2]])
                transposes_bf(hbb[b2], hT, b2 * 128)
            for pc in range(8):
                for s4 in range(4):
                    cd = cand[s4]
                    dma("sp", cd.ap[:], exo.ap[(s4 * SEG + tok0) // 256, pc * 384:(pc + 1) * 384, :].rearrange("(c p) t -> p c t", p=128), reads=[exo], writes=[cd])
                    dst = exs.ap[:, pc * 3:(pc + 1) * 3, :]
                    if s4 == 0:
                        op("dve", lambda cd=cd, dst=dst: nc.vector.tensor_scalar(out=dst, in0=cd.ap[:], scalar1=ohc[:, 0:1], scalar2=None, op0=ALU.mult), [cd, cst], [exs])
                    else:
                        op("dve", lambda cd=cd, dst=dst, s4=s4: nc.vector.scalar_tensor_tensor(out=dst, in0=cd.ap[:], scalar=ohc[:, s4:s4 + 1], in1=dst, op0=ALU.mult, op1=ALU.add), [cd, cst, exs], [exs])
            wi = 0
            for h in range(4):
                wb = wbuf[wi % 2]
                wi += 1
                wb4 = wb.ap[:, 0:4096].rearrange("p (a c n) -> p a c n", a=2, n=128)
                dma("sp", wb4[:, 0], v_wB1(2 * h), reads=[wBt], writes=[wb])
                dma("sp", wb4[:, 1], v_wB1(2 * h + 1), reads=[wBt], writes=[wb], multi=True)
                for c in range(2):
                    pb = PS[2 + c]
                    for kc in range(16):
                        op("pe", lambda kc=kc, pb=pb, wb4=wb4, c=c: nc.tensor.matmul(pb.ap[:, 0:256], lhsT=wb4[:, c, kc, :], rhs=hT.ap[:, kc, :], start=(kc == 0), stop=(kc == 15)), [wb, hT], [pb])
                    op("act", lambda pb=pb, c=c: nc.scalar.copy(out=xaq.ap[:, c, :], in_=pb.ap[:, 0:256]), [pb], [xaq])
                for mb in range(2):
                    pb = PS[4 + mb]
                    for c in range(2):
                        op("pe", lambda c=c, pb=pb, mb=mb, h=h: nc.tensor.matmul(pb.ap[:, 0:256], lhsT=mkT.ap[:, h * 2 + c, mb * 128:(mb + 1) * 128], rhs=xaq.ap[:, c, :], start=(c == 0), stop=(c == 1)), [mkT, xaq], [pb])
                    op("act", lambda pb=pb, mb=mb: nc.scalar.activation(out=Pm.ap[:, mb, :], in_=pb.ap[:, 0:256], func=AF.Exp, scale=1.0 / 16.0), [pb], [Pm])
                pbs = PS[6]
                for mb in range(2):
                    op("pe", lambda mb=mb: nc.tensor.matmul(pbs.ap[:, 0:256], lhsT=onesb.ap[:], rhs=Pm.ap[:, mb, :], start=(mb == 0), stop=(mb == 1)), [onesb, Pm], [pbs])
                op("dve", lambda: nc.vector.reciprocal(out=rcp.ap[:], in_=pbs.ap[:, 0:256]), [pbs], [rcp])
                for dvc in range(2):
                    pb = PS[2 + dvc]
                    for mb in range(2):
                        op("pe", lambda mb=mb, pb=pb, dvc=dvc, h=h: nc.tensor.matmul(pb.ap[:, 0:256], lhsT=mvv.ap[:, mb, h, dvc * 128:(dvc + 1) * 128], rhs=Pm.ap[:, mb, :], start=(mb == 0), stop=(mb == 1)), [mvv, Pm], [pb])
                    op("dve", lambda pb=pb, dvc=dvc, h=h: nc.vector.tensor_tensor(out=xaT.ap[:, h * 2 + dvc, :], in0=pb.ap[:, 0:256], in1=rcp.ap[:], op=ALU.mult), [pb, rcp], [xaT])
            for c in range(16):
                wg_ = wbuf[wi % 2]
                wi += 1
                wg4 = wg_.ap[:, 0:6144].rearrange("p (a c n) -> p a c n", a=3, n=128)
                for br in range(3):
                    dma("sp", wg4[:, br], v_wB1(8 + br * 16 + c), reads=[wBt], writes=[wg_], multi=(br > 0))
                wb_ = wbuf[wi % 2]
                wi += 1
                wb3 = wb_.ap[:, 0:4096].rearrange("p (c n) -> p c n", n=128)
                dma("sp", wb3[:, 0:16, :], v_wbr(c, 0, 16), reads=[wbrt], writes=[wb_])
                dma("sp", wb3[:, 16:32, :], v_wbr(c, 16, 16), reads=[wbrt], writes=[wb_], multi=True)
                for br in range(3):
                    pb = PS[br]
                    for kc in range(16):
                        op("pe", lambda kc=kc, pb=pb, br=br, wg4=wg4: nc.tensor.matmul(pb.ap[:, 0:256], lhsT=wg4[:, br, kc, :], rhs=hT.ap[:, kc, :], start=(kc == 0), stop=(kc == 15)), [wg_, hT], [pb])
                    op("act", lambda pb=pb, br=br, c=c: nc.scalar.activation(out=sig[br].ap[:], in_=pb.ap[:, 0:256], func=AF.Sigmoid, bias=fm.ap[:, br * 16 + c:br * 16 + c + 1]), [pb, fm], [sig[br]])
                lst = [(r * 4 + j, r * 6 + j) for r in range(4) for j in range(4)]
                for i_, (wk, ek) in enumerate(lst):
                    op("pe", lambda wk=wk, ek=ek, i_=i_, wb3=wb3: nc.tensor.matmul(PS[3].ap[:, 0:256], lhsT=wb3[:, wk, :], rhs=exs.ap[:, ek, :], start=(i_ == 0), stop=(i_ == 15)), [wb_, exs], [PS[3]])
                lst = [(16 + r * 2 + j, r * 6 + 4 + j) for r in range(4) for j in range(2)]
                for i_, (wk, ek) in enumerate(lst):
                    op("pe", lambda wk=wk, ek=ek, i_=i_, wb3=wb3: nc.tensor.matmul(PS[4].ap[:, 0:256], lhsT=wb3[:, wk, :], rhs=exs.ap[:, ek, :], start=(i_ == 0), stop=(i_ == 7)), [wb_, exs], [PS[4]])
                for kc in range(8):
                    op("pe", lambda kc=kc, wb3=wb3: nc.tensor.matmul(PS[5].ap[:, 0:256], lhsT=wb3[:, 24 + kc, :], rhs=xaT.ap[:, kc, :], start=(kc == 0), stop=(kc == 7)), [wb_, xaT], [PS[5]])
                op("dve", lambda: nc.vector.tensor_tensor(out=tm[0].ap[:], in0=PS[3].ap[:, 0:256], in1=sig[0].ap[:], op=ALU.mult), [PS[3], sig[0]], [tm[0]])
                op("dve", lambda: nc.vector.tensor_tensor(out=tm[1].ap[:], in0=PS[4].ap[:, 0:256], in1=sig[1].ap[:], op=ALU.mult), [PS[4], sig[1]], [tm[1]])
                op("pool", lambda: nc.gpsimd.tensor_tensor(out=tm[0].ap[:], in0=tm[0].ap[:], in1=tm[1].ap[:], op=ALU.add), [tm[0], tm[1]], [tm[0]])
                op("dve", lambda: nc.vector.tensor_tensor(out=tm[1].ap[:], in0=PS[5].ap[:, 0:256], in1=sig[2].ap[:], op=ALU.mult), [PS[5], sig[2]], [tm[1]])
                op("dve", lambda c=c: nc.vector.tensor_tensor(out=mT.ap[:, c, :], in0=tm[0].ap[:], in1=tm[1].ap[:], op=ALU.add), [tm[0], tm[1]], [mT])
            C.release(mk1)
            if KSTOP == "B1a":
                return RET()
            mk1 = C.mark()
            gffn = C.sb("gffn", [128, 1, D], F32)
            dma("sp", gffn.ap[:], gb_d.ap[:, 1:2, :], reads=[gb_d], writes=[gffn])
            wbuf = [C.sb(f"wbufb{i}", [128, 8192], BF16) for i in range(2)]
            h2f = C.sb("h2f", [128, D], F32)
            sqj = C.sb("sqj2", [128, D], BF16)
            h2Tf = C.sb("h2Tf", [128, 16, 128], F32)
            lg = C.sb("lg", [128, 36], F32)
            rt = C.sb("rt", [128, 160], F32)
            for ct in range(4):
                wb = wbuf[ct % 2]
                wb3 = wb.ap[:].rearrange("p (c n) -> p c n", n=512)
                dma("sp", wb3, v_wout(ct), reads=[woutt], writes=[wb])
                for b2 in range(2):
                    blk = hf * 2 + b2
                    pb = PS[2 + b2]
                    for kc in range(16):
                        op("pe", lambda kc=kc, pb=pb, wb3=wb3, b2=b2: nc.tensor.matmul(pb.ap[:], lhsT=mT.ap[:, kc, b2 * 128:(b2 + 1) * 128], rhs=wb3[:, kc, :], start=(kc == 0), stop=(kc == 15)), [wb, mT], [pb])
                    op("dve", lambda pb=pb, blk=blk, ct=ct: nc.vector.tensor_tensor(out=xt.ap[:, blk, ct * 512:(ct + 1) * 512], in0=pb.ap[:], in1=xt.ap[:, blk, ct * 512:(ct + 1) * 512], op=ALU.add), [pb], [xt])
            for b2 in range(2):
                blk = hf * 2 + b2
                st = nst()
                op("act", lambda blk=blk, st=st: nc.scalar.activation(out=sqj.ap[:], in_=xt.ap[:, blk, :], func=AF.Square, accum_out=st.ap[:, 0:1]), [xt, st], [sqj, st])
                op("act", lambda st=st: nc.scalar.activation(out=st.ap[:, 1:2], in_=st.ap[:, 0:1], func=AF.Ln, scale=1.0 / D, bias=sc.ap[:, 2:3]), [st, sc], [st])
                op("act", lambda st=st: nc.scalar.activation(out=st.ap[:, 2:3], in_=st.ap[:, 1:2], func=AF.Exp, scale=-0.5), [st], [st])
                op("dve", lambda blk=blk, st=st: nc.vector.scalar_tensor_tensor(out=h2f.ap[:], in0=xt.ap[:, blk, :], scalar=st.ap[:, 2:3], in1=gffn.ap[:, 0, :], op0=ALU.mult, op1=ALU.mult), [xt, st, gffn], [h2f])
                for q4 in range(4):
                    pb = PS[4 + q4]
                    for c4 in range(4):
                        c = q4 * 4 + c4
                        op("pe", lambda c=c, c4=c4, pb=pb: nc.tensor.transpose(out=pb.ap[:, c4 * 128:(c4 + 1) * 128], in_=h2f.ap[:, c * 128:(c + 1) * 128], identity=identf.ap[:]), [h2f, identf], [pb])
                    src = pb.ap[:].rearrange("p (c t) -> p c t", c=4)
                    op("act", lambda src=src, q4=q4: nc.scalar.copy(out=h2Tf.ap[:, q4 * 4:(q4 + 1) * 4, :], in_=src), [pb], [h2Tf])
                    op("dve", lambda src=src, q4=q4, blk=blk: nc.vector.tensor_copy(out=h2T.ap[:, q4 * 4:(q4 + 1) * 4, blk * 128:(blk + 1) * 128], in_=src), [pb], [h2T])
                pbr = PS[2]
                for kc in range(16):
                    op("pe", lambda kc=kc: nc.tensor.matmul(pbr.ap[:, 0:36], lhsT=h2Tf.ap[:, kc, :], rhs=wr_sb.ap[:, kc, :], start=(kc == 0), stop=(kc == 15)), [h2Tf, wr_sb], [pbr])
                BIG = 1.0e4
                A = rt.ap
                V = nc.vector
                op("dve", lambda: V.tensor_tensor(out=lg.ap[:], in0=pbr.ap[:, 0:36], in1=brt.ap[:], op=ALU.add), [pbr, brt], [lg])
                seq = [
                    lambda: V.tensor_reduce(out=A[:, 0:1], in_=lg.ap[:, 0:4], axis=AX.X, op=ALU.max),
                    lambda: V.tensor_scalar(out=A[:, 1:2], in0=A[:, 0:1], scalar1=-1.0, scalar2=None, op0=ALU.mult),
                    lambda: V.tensor_scalar(out=A[:, 8:12], in0=lg.ap[:, 0:4], scalar1=A[:, 0:1], scalar2=None, op0=ALU.is_ge),
                    lambda: V.tensor_scalar(out=A[:, 12:16], in0=A[:, 8:12], scalar1=BIG, scalar2=-BIG, op0=ALU.mult, op1=ALU.add),
                ]
                for f_ in seq:
                    op("dve", f_, [lg, rt], [rt])
                op("act", lambda: nc.scalar.activation(out=A[:, 16:20], in_=lg.ap[:, 0:4], func=AF.Exp, bias=A[:, 1:2]), [lg, rt], [rt])
                seq = [
                    lambda: V.tensor_reduce(out=A[:, 2:3], in_=A[:, 16:20], axis=AX.X, op=ALU.add),
                    lambda: V.reciprocal(out=A[:, 3:4], in_=A[:, 2:3]),
                ]
                for g4 in range(4):
                    seq.append(lambda g4=g4: V.tensor_scalar(out=A[:, 32 + g4 * 8:40 + g4 * 8], in0=lg.ap[:, 4 + g4 * 8:12 + g4 * 8], scalar1=A[:, 12 + g4:13 + g4], scalar2=None, op0=ALU.add))
                seq += [
                    lambda: V.tensor_reduce(out=A[:, 4:5], in_=A[:, 32:64], axis=AX.X, op=ALU.max),
                    lambda: V.tensor_scalar(out=A[:, 64:96], in0=A[:, 32:64], scalar1=A[:, 4:5], scalar2=None, op0=ALU.is_ge),
                    lambda: V.scalar_tensor_tensor(out=A[:, 96:128], in0=A[:, 64:96], scalar=-BIG, in1=A[:, 32:64], op0=ALU.mult, op1=ALU.add),
                    lambda: V.tensor_reduce(out=A[:, 5:6], in_=A[:, 96:128], axis=AX.X, op=ALU.max),
                    lambda: V.tensor_scalar(out=A[:, 128:160], in0=A[:, 96:128], scalar1=A[:, 5:6], scalar2=None, op0=ALU.is_ge),
                    lambda: V.tensor_tensor(out=A[:, 6:7], in0=A[:, 5:6], in1=A[:, 4:5], op=ALU.subtract),
                ]
                for f_ in seq:
                    op("dve", f_, [lg, rt], [rt])
                op("act", lambda: nc.scalar.activation(out=A[:, 7:8], in_=A[:, 6:7], func=AF.Exp), [rt], [rt])
                seq = [
                    lambda: V.tensor_scalar(out=A[:, 20:21], in0=A[:, 7:8], scalar1=1.0, scalar2=None, op0=ALU.add),
                    lambda: V.reciprocal(out=A[:, 21:22], in_=A[:, 20:21]),
                    lambda: V.tensor_tensor(out=A[:, 22:23], in0=A[:, 21:22], in1=A[:, 3:4], op=ALU.mult),
                    lambda: V.tensor_tensor(out=A[:, 23:24], in0=A[:, 22:23], in1=A[:, 7:8], op=ALU.mult),
                ]
                for f_ in seq:
                    op("dve", f_, [rt], [rt])
                op("dve", lambda blk=blk: V.tensor_scalar(out=rw.ap[:, blk, :], in0=A[:, 64:96], scalar1=A[:, 22:23], scalar2=None, op0=ALU.mult), [rt], [rw])
                op("dve", lambda blk=blk: V.scalar_tensor_tensor(out=rw.ap[:, blk, :], in0=A[:, 128:160], scalar=A[:, 23:24], in1=rw.ap[:, blk, :], op0=ALU.mult, op1=ALU.add), [rt], [rw])
            if dbg and "x1_dbg" in dbg:
                for b2 in range(2):
                    blk = hf * 2 + b2
                    dma("sp", x1_dbg.ap[tokb + blk * 128:tokb + (blk + 1) * 128, :], xt.ap[:, blk, :], reads=[xt], writes=[x1_dbg], multi=True, owner=x1_dbg)
                    dma("sp", rw_dbg.ap[tokb + blk * 128:tokb + (blk + 1) * 128, :], rw.ap[:, blk, :], reads=[rw], writes=[rw_dbg], multi=True, owner=rw_dbg)
            C.release(mk1)
            if KSTOP == "B1b":
                return RET()
        mk1 = C.mark()
        gfin = C.sb("gfin", [128, 1, D], F32)
        dma("sp", gfin.ap[:], gb_d.ap[:, 2:3, :], reads=[gb_d], writes=[gfin])
        wgb = C.sb("wgb", [128, 16, DEXP], BF16)
        wub = C.sb("wub", [128, 16, DEXP], BF16)
        wdb = C.sb("wdb", [128, 6, D], BF16)
        actT = C.sb("actT", [128, 6, TB], BF16)
        sil = [C.sb(f"sil{i}", [128, TB], F32) for i in range(2)]
        sqj = C.sb("sqj3", [128, D], BF16)
        ob = [C.sb(f"ob{i}", [128, D], F32) for i in range(2)]
        psi = 0
        for e in range(NEXP):
            for k0 in (0, 8):
                dma("sp", wgb.ap[:, k0:k0 + 8, :], weg_bf.ap[e, k0 * 128:(k0 + 8) * 128, :].rearrange("(c p) n -> p c n", p=128), reads=[weg_bf], writes=[wgb], multi=(k0 > 0))
                dma("sp", wub.ap[:, k0:k0 + 8, :], weu_bf.ap[e, k0 * 128:(k0 + 8) * 128, :].rearrange("(c p) n -> p c n", p=128), reads=[weu_bf], writes=[wub], multi=(k0 > 0))
            dma("sp", wdb.ap[:, 0:5, :], wed_bf.ap[e, 0:640, :].rearrange("(c p) n -> p c n", p=128), reads=[wed_bf], writes=[wdb])
            dma("sp", wdb.ap[0:64, 5, :], wed_bf.ap[e, 640:704, :], reads=[wed_bf], writes=[wdb], multi=True)
            for dc in range(6):
                M = 128 if dc < 5 else 64
                pa = PS[(psi % 2) * 2]
                pu = PS[(psi % 2) * 2 + 1]
                sl = sil[psi % 2]
                psi += 1
                for kc in range(16):
                    op("pe", lambda kc=kc, pa=pa, dc=dc, M=M: nc.tensor.matmul(pa.ap[0:M, :], lhsT=wgb.ap[:, kc, dc * 128:dc * 128 + M], rhs=h2T.ap[:, kc, :], start=(kc == 0), stop=(kc == 15)), [wgb, h2T], [pa])
                for kc in range(16):
                    op("pe", lambda kc=kc, pu=pu, dc=dc, M=M: nc.tensor.matmul(pu.ap[0:M, :], lhsT=wub.ap[:, kc, dc * 128:dc * 128 + M], rhs=h2T.ap[:, kc, :], start=(kc == 0), stop=(kc == 15)), [wub, h2T], [pu])
                op("act", lambda pa=pa, sl=sl, M=M: nc.scalar.activation(out=sl.ap[0:M, :], in_=pa.ap[0:M, :], func=AF.Silu), [pa], [sl])
                op("dve", lambda pu=pu, sl=sl, M=M, dc=dc: nc.vector.tensor_tensor(out=actT.ap[0:M, dc, :], in0=pu.ap[0:M, :], in1=sl.ap[0:M, :], op=ALU.mult), [pu, sl], [actT])
            for blk in range(4):
                for ct in range(4):
                    pb = PS[4 + (psi % 4)]
                    psi += 1
                    for dc in range(6):
                        M = 128 if dc < 5 else 64
                        op("pe", lambda dc=dc, M=M, pb=pb, blk=blk, ct=ct: nc.tensor.matmul(pb.ap[:], lhsT=actT.ap[0:M, dc, blk * 128:(blk + 1) * 128], rhs=wdb.ap[0:M, dc, ct * 512:(ct + 1) * 512], start=(dc == 0), stop=(dc == 5)), [actT, wdb], [pb])
                    op("dve", lambda pb=pb, blk=blk, ct=ct, e=e: nc.vector.scalar_tensor_tensor(out=xt.ap[:, blk, ct * 512:(ct + 1) * 512], in0=pb.ap[:], scalar=rw.ap[:, blk, e:e + 1], in1=xt.ap[:, blk, ct * 512:(ct + 1) * 512], op0=ALU.mult, op1=ALU.add), [pb, rw], [xt])
        for blk in range(4):
            st = nst()
            o_ = ob[blk % 2]
            op("act", lambda blk=blk, st=st: nc.scalar.activation(out=sqj.ap[:], in_=xt.ap[:, blk, :], func=AF.Square, accum_out=st.ap[:, 0:1]), [xt, st], [sqj, st])
            op("act", lambda st=st: nc.scalar.activation(out=st.ap[:, 1:2], in_=st.ap[:, 0:1], func=AF.Ln, scale=1.0 / D, bias=sc.ap[:, 2:3]), [st, sc], [st])
            op("act", lambda st=st: nc.scalar.activation(out=st.ap[:, 2:3], in_=st.ap[:, 1:2], func=AF.Exp, scale=-0.5), [st], [st])
            op("dve", lambda blk=blk, st=st, o_=o_: nc.vector.scalar_tensor_tensor(out=o_.ap[:], in0=xt.ap[:, blk, :], scalar=st.ap[:, 2:3], in1=gfin.ap[:, 0, :], op0=ALU.mult, op1=ALU.mult), [xt, st, gfin], [o_])
            dma("sp", out_d.ap[tokb + blk * 128:tokb + (blk + 1) * 128, :], o_.ap[:], reads=[o_], writes=[out_d], multi=True, owner=o_)
        C.release(mk1)
    C.barrier()
    return nc, C, dict(qT_s=qT_s, kT_s=kT_s, v_s=v_s, mq_s=mq_s, mk_s=mk_s, mv_s=mv_s, mo_s=mo_s, gt_s=gt_s, exi=exi, exo=exo, out=out_d)


def _pack_inputs(inp, S):
    f32 = np.float32
    SEG = S // 4
    w_in = np.asarray(inp["w_in"][0], f32)
    x = np.asarray(inp["x"], f32)[:, :S]
    mem = np.asarray(inp["mem"], f32)
    wbd = np.asarray(inp["w_branch_diff"][0], f32)
    rows = []
    for r in range(4):
        for j in range(4):
            h = HEAD_PAIRS[r][j // 2]
            r0 = h * 256 + (j % 2) * 128
            rows.append(wbd[r0:r0 + 128])
    wbr = np.concatenate(rows + [np.asarray(inp["w_branch_mlstm"][0], f32), np.asarray(inp["w_branch_cross"][0], f32)], axis=0)
    wB = np.ascontiguousarray(w_in[:, 10248:17416])
    wr = np.concatenate([np.asarray(inp["w_router_group"][0], f32), np.asarray(inp["w_router_expert"][0], f32)], axis=1)
    gbv = np.stack([np.asarray(inp[k], f32).reshape(-1) for k in ("g_mix", "g_ffn", "g_final", "g_mem")], 0)
    gb = np.ascontiguousarray(np.broadcast_to(gbv[None], (128, 4, D)))
    lamv = np.concatenate([np.asarray(inp[k], f32).reshape(-1) for k in ("lam_q1", "lam_k1", "lam_q2", "lam_k2")])
    lam = np.ascontiguousarray(np.broadcast_to(lamv[None], (128, 512)))
    brv = np.concatenate([np.asarray(inp["b_router_group"], f32).reshape(-1), np.asarray(inp["b_router_expert"], f32).reshape(-1)])
    brt = np.ascontiguousarray(np.broadcast_to(brv[None], (128, 36)))
    conv_w = np.asarray(inp["conv_w"][0], f32)
    conv_b = np.asarray(inp["conv_b"][0], f32)
    b_gate = np.asarray(inp["b_gate"][0], f32)
    gdh = np.asarray(inp["g_diff_head"][0], f32)
    gmh = np.asarray(inp["g_mlstm_head"][0], f32)
    p = np.arange(128)
    maps = []
    for c in range(NCORES):
        b, g = c // 4, c % 4
        hs = HEAD_PAIRS[g]
        hb = np.concatenate([gdh[hs[0]], gdh[hs[1]], gmh[g]])
        hb = np.ascontiguousarray(np.broadcast_to(hb[None], (128, 768)))
        fm = np.zeros((128, 64), f32)
        fm[:, 0:48] = b_gate.reshape(48, 128).T
        for j in range(4):
            base = (g * 256 + j * 128) if j < 2 else (1024 + g * 256 + (j - 2) * 128)
            for tap in range(4):
                fm[:, 48 + j * 4 + tap] = conv_w[tap, base:base + 128]
        cst = np.zeros((128, 1024), f32)
        cst[:, 0:128] = np.eye(128, dtype=f32)
        cst[:, 128:256] = (p[:, None] <= p[None, :]).astype(f32)
        cst[:, 256:384] = np.where(p[:, None] <= p[None, :], 0.0, -30000.0).astype(f32)
        for hl in range(2):
            sl = SLOPES[hs[hl]]
            idx = np.arange(130)
            cst[:, 384 + hl * 130:384 + (hl + 1) * 130] = sl * (p[:, None] - 128 + 128 * (idx[None, :] - 128))
        for j in range(4):
            base = (g * 256 + j * 128) if j < 2 else (1024 + g * 256 + (j - 2) * 128)
            cst[:, 644 + j] = conv_b[base:base + 128]
        cst[:, 648] = np.asarray(inp["b_igate"], f32).reshape(-1)[g]
        cst[:, 649] = np.asarray(inp["b_fgate"], f32).reshape(-1)[g]
        cst[:, 652 + g] = 1.0
        maps.append({
            "x": np.ascontiguousarray(x[b]),
            "xseg": np.ascontiguousarray(x[b, g * SEG:(g + 1) * SEG]),
            "mem": np.ascontiguousarray(mem[b]),
            "wA": np.ascontiguousarray(w_in[:, head_cols(g)]),
            "wB": wB,
            "wmem": np.asarray(inp["w_mem_kv"][0], f32),
            "wbr": wbr,
            "wout": np.asarray(inp["w_out"][0], f32),
            "wr": wr,
            "weg": np.asarray(inp["w_expert_gate"][0], f32),
            "weu": np.asarray(inp["w_expert_up"][0], f32),
            "wed": np.asarray(inp["w_expert_down"][0], f32),
            "gb": gb, "hb": hb, "fm": fm, "lam": lam, "brt": brt, "cst": cst,
        })
    return maps


def run(inp, S, dbg=False, trace=False, full=True):
    nc, C, bufs = build(S, dbg=dbg, full=full)
    maps = _pack_inputs(inp, S)
    if not full:
        for m in maps:
            for k in ("weg", "weu", "wed"):
                m.pop(k)
    res = run_bass_kernel_spmd(nc, maps, core_ids=list(range(NCORES)), trace=trace)
    return res


def kernel(**inputs):
    S = 16384
    res = run(inputs, S)
    SEG = S // 4
    out = np.zeros((2, S, D), np.float32)
    for c in range(NCORES):
        b, g = c // 4, c % 4
        out[b, g * SEG:(g + 1) * SEG] = res.results[c]["out"]
    return out
```
